# Optimizing a Trainium2 kernel written in Bass

```python
import jax, jax.numpy as jnp
from jax import lax
import numpy as np

D_MODEL = 1024
BATCH = 32
SEQ = 2048
DEPTH = 1

CTX_LEN = 256
GRID_W = 64
N_MOD = 6
EPS = 1e-6
A_HEADS = 8
A_HEAD_DIM = 128
A_WIDTH = A_HEADS * A_HEAD_DIM
A_CHUNK = 64
B_GROUPS = 8
B_GROUP_DIM = 128
B_WIDTH = B_GROUPS * B_GROUP_DIM
B_CHUNK = 128
B_ROWS_PER_CHUNK = B_CHUNK // GRID_W
N_EXPERTS = 32
TOP_K = 4
D_EXPERT = 1024
SWIGLU_LIMIT = 7.0
SWIGLU_ALPHA = 1.702
MOE_BLOCK = 256
SPLITS = (A_WIDTH, A_WIDTH, A_WIDTH, A_WIDTH, A_WIDTH, B_WIDTH, B_WIDTH, D_MODEL, D_MODEL)
IN_WIDTH = 5 * A_WIDTH + 2 * B_WIDTH + 2 * D_MODEL

kernel_name = "hybrid_hgrn2_chunkmlp_moe_dit_block"


def rms_norm(x, g):
    xf = x.astype(jnp.float32)
    y = xf * lax.rsqrt(jnp.mean(xf * xf, axis=-1, keepdims=True) + EPS)
    return (y * g).astype(x.dtype)


def modulate(x, g, shift, scale):
    return rms_norm(x, g) * (1.0 + scale) + shift


def project(h, w_in, lb_f, lb_b):
    bsz, t, _ = h.shape
    z = h @ w_in
    q, i, f_fwd, f_bwd, og, u, v, gate_a, gate_b = jnp.split(
        z, np.cumsum(SPLITS)[:-1].tolist(), axis=-1)

    def heads(a):
        return a.astype(jnp.float32).reshape(bsz, t, A_HEADS, A_HEAD_DIM)

    def decay(f_logit, lb):
        f = lb + (1.0 - lb) * jax.nn.sigmoid(f_logit.astype(jnp.float32))
        return heads(1.0 - f), heads(jnp.log(f))

    k_f, logf_f = decay(f_fwd, lb_f)
    k_b, logf_b = decay(f_bwd, lb_b)
    return (heads(jax.nn.silu(q)), heads(i), k_f, logf_f, k_b, logf_b, og,
            jax.nn.gelu(u, approximate=False), jax.nn.gelu(v, approximate=False),
            gate_a, gate_b)


def gla_chunked(q, k, v, logf, s0):
    bsz, t, nh, dk = q.shape
    dv = v.shape[-1]
    n = t // A_CHUNK

    def to_chunks(a):
        return jnp.swapaxes(a.reshape(bsz, n, A_CHUNK, nh, a.shape[-1]), 0, 1)

    lower = jnp.tril(jnp.ones((A_CHUNK, A_CHUNK), dtype=bool))

    def step(s, xs):
        qc, kc, vc, gc = xs
        b = jnp.cumsum(gc, axis=1)
        b_ref = b[:, A_CHUNK // 2 - 1:A_CHUNK // 2]
        b_end = b[:, -1]
        scores = jnp.einsum('bthd,bshd->bhts', qc * jnp.exp(b - b_ref), kc * jnp.exp(b_ref - b))
        scores = jnp.where(lower, scores, 0.0)
        o = (jnp.einsum('bhts,bshv->bthv', scores, vc)
             + jnp.einsum('bthd,bhdv->bthv', qc * jnp.exp(b), s))
        s = (jnp.exp(b_end)[..., None] * s
             + jnp.einsum('bshd,bshv->bhdv', kc * jnp.exp(b_end[:, None] - b), vc))
        return s, o

    _, o = lax.scan(step, s0, (to_chunks(q), to_chunks(k), to_chunks(v), to_chunks(logf)))
    return jnp.swapaxes(o, 0, 1).reshape(bsz, t, nh, dv)


def final_state(k, v, logf):
    tail = lax.cumsum(logf, axis=1, reverse=True) - logf
    return jnp.einsum('bshd,bshv->bhdv', k * jnp.exp(tail), v)


def context_states(p):
    q, i, k_f, logf_f, k_b, logf_b = p[:6]
    rev = lambda a: jnp.flip(a, axis=1)
    return final_state(k_f, i, logf_f), final_state(rev(k_b), rev(i), rev(logf_b))


def chunk_mlp(u, v, w_s, b_s, n_chunks):
    bsz, t, _ = v.shape
    vf = v.astype(jnp.float32)
    mu = jnp.mean(vf, axis=-1, keepdims=True)
    var = jnp.mean(jnp.square(vf - mu), axis=-1, keepdims=True)
    vn = ((vf - mu) * lax.rsqrt(var + EPS)).astype(v.dtype)
    vn = vn.reshape(bsz, n_chunks, B_CHUNK, B_GROUPS, B_GROUP_DIM)
    s = jnp.einsum('gtp,bnpgc->bntgc', w_s, vn) + b_s.T[:, :, None]
    return u * s.reshape(bsz, t, B_WIDTH)


def token_mix(p, s_fwd0, s_bwd0, gnorm_g, w_s, b_s, w_branch_a, w_branch_b, w_out, n_chunks):
    q, i, k_f, logf_f, k_b, logf_b, og, u, v, gate_a, gate_b = p
    bsz, t = og.shape[:2]
    rev = lambda a: jnp.flip(a, axis=1)
    o = (gla_chunked(q, k_f, i, logf_f, s_fwd0)
         + rev(gla_chunked(rev(q), rev(k_b), rev(i), rev(logf_b), s_bwd0)))
    y_a = rms_norm(o, gnorm_g).reshape(bsz, t, A_WIDTH).astype(og.dtype) * jax.nn.silu(og)
    y_b = chunk_mlp(u, v, w_s, b_s, n_chunks)
    merged = (jax.nn.sigmoid(gate_a) * (y_a @ w_branch_a)
              + jax.nn.sigmoid(gate_b) * (y_b @ w_branch_b))
    return merged @ w_out


def moe_ffn(h, w_router, b_router, w1, b1, w2, b2):
    bsz, t, d = h.shape
    hf = h.reshape(bsz * t, d)
    n_tok = hf.shape[0]
    n_assign = n_tok * TOP_K
    logits = (hf @ w_router + b_router).astype(jnp.float32)
    top_logit, top_e = lax.top_k(logits, TOP_K)
    top_w = jax.nn.softmax(top_logit, axis=-1)
    e_flat = top_e.reshape(-1).astype(jnp.int32)
    w_flat = top_w.reshape(-1)
    e_sorted, order = lax.sort((e_flat, jnp.arange(n_assign, dtype=jnp.int32)),
                               num_keys=1, is_stable=True)
    counts = jnp.bincount(e_flat, length=N_EXPERTS)
    starts = jnp.cumsum(counts) - counts
    padded = (counts + MOE_BLOCK - 1) // MOE_BLOCK * MOE_BLOCK
    pad_ends = jnp.cumsum(padded)
    pad_starts = pad_ends - padded
    dest = pad_starts[e_sorted] + (jnp.arange(n_assign, dtype=jnp.int32) - starts[e_sorted])
    n_blocks = -(-n_assign // MOE_BLOCK) + N_EXPERTS
    cap = n_blocks * MOE_BLOCK
    slot_tok = jnp.full((cap,), n_tok, jnp.int32).at[dest].set(order // TOP_K)
    slot_w = jnp.zeros((cap,), jnp.float32).at[dest].set(w_flat[order])
    block_e = jnp.clip(jnp.searchsorted(pad_ends, jnp.arange(n_blocks) * MOE_BLOCK, side='right'),
                       0, N_EXPERTS - 1)
    x_pad = jnp.concatenate([hf, jnp.zeros((1, d), hf.dtype)], axis=0)
    xb = x_pad[slot_tok].reshape(n_blocks, MOE_BLOCK, d)

    def expert_block(args):
        xblk, e = args
        z = xblk @ w1[e] + b1[e]
        gate = jnp.minimum(z[:, :D_EXPERT], SWIGLU_LIMIT)
        lin = jnp.clip(z[:, D_EXPERT:], -SWIGLU_LIMIT, SWIGLU_LIMIT)
        return (gate * jax.nn.sigmoid(SWIGLU_ALPHA * gate) * (lin + 1.0)) @ w2[e] + b2[e]

    y = lax.map(expert_block, (xb, block_e)).reshape(cap, d)
    out = jnp.zeros((n_tok + 1, d), hf.dtype).at[slot_tok].add(
        (y * slot_w[:, None]).astype(hf.dtype))[:n_tok]
    return out.reshape(bsz, t, d)


def setup_inputs(seed: int = 0) -> dict:
    key = jax.random.key(seed)
    ks = jax.random.split(key, 24)
    nrm = lambda k, shape, s: s * jax.random.normal(k, shape, jnp.float32)
    L, E, F = DEPTH, N_EXPERTS, D_EXPERT
    return {
        'x': nrm(ks[0], (BATCH, SEQ, D_MODEL), 1.0),
        'c': nrm(ks[1], (BATCH, D_MODEL), 1.0),
        'ctx': nrm(ks[2], (BATCH, CTX_LEN, D_MODEL), 1.0),
        'c_ctx': nrm(ks[3], (D_MODEL,), 1.0),
        'norm1_g': 1.0 + nrm(ks[4], (L, D_MODEL), 0.02),
        'norm2_g': 1.0 + nrm(ks[5], (L, D_MODEL), 0.02),
        'w_mod': nrm(ks[6], (L, D_MODEL, N_MOD * D_MODEL), 0.5 * D_MODEL ** -0.5),
        'b_mod': nrm(ks[7], (L, N_MOD * D_MODEL), 0.02),
        'w_in': nrm(ks[8], (L, D_MODEL, IN_WIDTH), D_MODEL ** -0.5),
        'lb_fwd': nrm(ks[9], (L + 1, A_WIDTH), 0.1),
        'lb_bwd': nrm(ks[10], (L + 1, A_WIDTH), 0.1),
        'gnorm_g': 1.0 + nrm(ks[11], (L, A_HEAD_DIM), 0.02),
        'w_s': nrm(ks[12], (L, B_GROUPS, B_CHUNK, B_CHUNK), B_CHUNK ** -0.5),
        'b_s': nrm(ks[13], (L, B_GROUPS, B_CHUNK), 0.02),
        'w_branch_a': nrm(ks[14], (L, A_WIDTH, D_MODEL), A_WIDTH ** -0.5),
        'w_branch_b': nrm(ks[15], (L, B_WIDTH, D_MODEL), B_WIDTH ** -0.5),
        'w_out': nrm(ks[16], (L, D_MODEL, D_MODEL), D_MODEL ** -0.5),
        'w_router': nrm(ks[17], (L, D_MODEL, E), D_MODEL ** -0.5),
        'b_router': nrm(ks[18], (L, E), 0.01),
        'w1': nrm(ks[19], (L, E, D_MODEL, 2 * F), D_MODEL ** -0.5),
        'b1': nrm(ks[20], (L, E, 2 * F), 0.02),
        'w2': nrm(ks[21], (L, E, F, D_MODEL), F ** -0.5),
        'b2': nrm(ks[22], (L, E, D_MODEL), 0.02),
        'final_g': 1.0 + nrm(ks[23], (D_MODEL,), 0.02),
    }


def reference(x, c, ctx, c_ctx, norm1_g, norm2_g, w_mod, b_mod, w_in, lb_fwd, lb_bwd, gnorm_g,
              w_s, b_s, w_branch_a, w_branch_b, w_out, w_router, b_router, w1, b1, w2, b2, final_g):
    bsz, t, d = x.shape
    rows = t // GRID_W
    lat_chunks = rows // B_ROWS_PER_CHUNK
    ctx_chunks = ctx.shape[1] // B_CHUNK
    lb_f_all = jnp.cumsum(jax.nn.softmax(lb_fwd.astype(jnp.float32), axis=0), axis=0)
    lb_b_all = jnp.cumsum(jax.nn.softmax(lb_bwd.astype(jnp.float32), axis=0), axis=0)
    for layer in range(DEPTH):
        last = layer == DEPTH - 1
        mod = jax.nn.silu(c) @ w_mod[layer] + b_mod[layer]
        mod_c = jax.nn.silu(c_ctx) @ w_mod[layer] + b_mod[layer]
        sh1, sc1, g1, sh2, sc2, g2 = jnp.split(mod[:, None, :], N_MOD, axis=-1)
        csh1, csc1, cg1, csh2, csc2, cg2 = jnp.split(mod_c, N_MOD)
        p_lat = project(modulate(x, norm1_g[layer], sh1, sc1), w_in[layer],
                        lb_f_all[layer], lb_b_all[layer])
        p_ctx = project(modulate(ctx, norm1_g[layer], csh1, csc1), w_in[layer],
                        lb_f_all[layer], lb_b_all[layer])
        s_fwd, s_bwd = context_states(p_ctx)
        mix_w = (gnorm_g[layer], w_s[layer], b_s[layer], w_branch_a[layer], w_branch_b[layer],
                 w_out[layer])
        ffn_w = (w_router[layer], b_router[layer], w1[layer], b1[layer], w2[layer], b2[layer])
        x = x + g1 * token_mix(p_lat, s_fwd, s_bwd, *mix_w, lat_chunks)
        x = x + g2 * moe_ffn(modulate(x, norm2_g[layer], sh2, sc2), *ffn_w)
        if not last:
            zero = jnp.zeros_like(s_fwd)
            ctx = ctx + cg1 * token_mix(p_ctx, zero, zero, *mix_w, ctx_chunks)
            ctx = ctx + cg2 * moe_ffn(modulate(ctx, norm2_g[layer], csh2, csc2), *ffn_w)
    return rms_norm(x, final_g)
```

```python
from contextlib import ExitStack
import numpy as np
import concourse.bass as bass
import concourse.mybir as mybir
from concourse.bass_utils import run_bass_kernel_spmd

F32 = mybir.dt.float32
BF16 = mybir.dt.bfloat16
I32 = mybir.dt.int32
AF = mybir.ActivationFunctionType
ALU = mybir.AluOpType

ENGS = ("tensor", "vector", "scalar", "gpsimd", "sync")
D = 1024
KC = 8
H = 8
NE = 32
EPS = 1e-6
LIMIT = 7.0
ALPHA = 1.702


class Buf:
    __slots__ = ("name", "w", "r")

    def __init__(self, name=""):
        self.name = name
        self.w = None
        self.r = {}


class Prog:
    def __init__(self, nc):
        self.nc = nc
        self.q = {e: [] for e in ENGS}
        self.clock = {e: 0 for e in ENGS}
        self.seen = {e: {} for e in ENGS}
        self.dma_count = {}
        self.n_dma_sems = 0

    def new_dma_sem(self):
        k = "dma%d" % self.n_dma_sems
        self.n_dma_sems += 1
        self.dma_count[k] = 0
        return k

    def _need(self, eng, deps):
        for (k, v) in deps:
            if k == eng and eng == "tensor":
                continue
            if self.seen[eng].get(k, 0) >= v:
                continue
            if k in self.dma_count:
                v = self.dma_count[k]
            if self.seen[eng].get(k, 0) < v:
                self.seen[eng][k] = v
                self.q[eng].append(("wait", k, v))

    def _deps(self, reads, writes):
        deps = []
        for b in reads:
            if b.w is not None:
                deps.append(b.w)
        for b in writes:
            if b.w is not None:
                deps.append(b.w)
            deps.extend(b.r.items())
        return deps

    def op(self, eng, fn, reads=(), writes=()):
        self._need(eng, self._deps(reads, writes))
        self.clock[eng] += 1
        tok = (eng, self.clock[eng])
        self.q[eng].append(("op", fn, eng))
        for b in reads:
            if b.r.get(eng, 0) < tok[1]:
                b.r[eng] = tok[1]
        for b in writes:
            b.w = tok
            b.r = {}
        return tok

    def dma(self, eng, fn, sem, reads=(), writes=()):
        self._need(eng, self._deps(reads, writes))
        self.dma_count[sem] += 16
        tok = (sem, self.dma_count[sem])
        self.q[eng].append(("dma", fn, sem))
        for b in reads:
            if b.r.get(sem, 0) < tok[1]:
                b.r[sem] = tok[1]
        for b in writes:
            b.w = tok
            b.r = {}
        return tok

    def barrier(self):
        toks = [(e, self.clock[e]) for e in ENGS if self.clock[e] > 0] + [(k, v) for k, v in self.dma_count.items() if v > 0]
        for e in ENGS:
            self._need(e, [t for t in toks if t[0] != e])

    def wait_all(self, eng, bufs):
        self._need(eng, [b.w for b in bufs if b.w is not None])

    def emit(self, stack):
        nc = self.nc
        sems = {}
        for e in ENGS:
            sems[e] = stack.enter_context(nc.semaphore("s_" + e))
        for k in self.dma_count:
            sems[k] = stack.enter_context(nc.semaphore("s_" + k))
        block = stack.enter_context(nc.Block())
        q = self.q

        def replay(ename):
            def body(eng):
                for item in q[ename]:
                    if item[0] == "wait":
                        eng.wait_ge(sems[item[1]], item[2])
                    elif item[0] == "op":
                        item[1](eng).then_inc(sems[item[2]], 1)
                    else:
                        item[1](eng).then_inc(sems[item[2]], 16)
            return body

        block.tensor(replay("tensor"))
        block.vector(replay("vector"))
        block.scalar(replay("scalar"))
        block.gpsimd(replay("gpsimd"))
        block.sync(replay("sync"))


def build_program(NB, T, CTXL, debug=False):
    nc = bass.Bass("TRN2", target_bir_lowering=False)
    R = NB + 1
    NTOK = NB * T
    NTILE = NTOK // 128
    NCH_B = T // 128
    ST = 512
    assert T % ST == 0 and CTXL % 128 == 0 and CTXL <= ST
    NST = T // ST
    TG = 512
    NTG = NTOK // TG

    def din(name, shape, dt=F32):
        return nc.dram_tensor(name, list(shape), dt, kind="ExternalInput").ap()

    x_d = din("x", [NB, T, D])
    ctx_d = din("ctx", [NB, CTXL, D])
    cc_d = din("cc", [R, D])
    n1g_d = din("norm1_g", [1, D]); n2g_d = din("norm2_g", [1, D])
    wmod_d = din("w_mod", [1, D, 6 * D]); bmod_d = din("b_mod", [1, 6 * D])
    win_d = din("w_in", [1, D, 9 * D])
    lbf_d = din("lb_fwd", [2, D]); lbb_d = din("lb_bwd", [2, D])
    gng_d = din("gnorm_g", [1, 128])
    ws_d = din("w_s", [1, 8, 128, 128]); bs_d = din("b_s", [1, 8, 128])
    wba_d = din("w_branch_a", [1, D, D]); wbb_d = din("w_branch_b", [1, D, D]); wout_d = din("w_out", [1, D, D])
    wr_d = din("w_router", [1, D, NE]); br_d = din("b_router", [1, NE])
    w1_d = din("w1", [1, NE, D, 2 * D]); b1_d = din("b1", [1, NE, 2 * D])
    w2_d = din("w2", [1, NE, D, D]); b2_d = din("b2", [1, NE, D])
    fg_d = din("final_g", [D])
    cid_d = din("c_ident", [128, 128]); cmk_d = din("c_mask", [128, 256]); csel_d = din("c_sel", [8, 8 * 128])
    BLK_ = 512
    NBLK_ = (4 * NTOK) // BLK_ + NE
    JMAX_ = max(1, NTOK // BLK_)
    clst_d = din("c_lst", [128, 128]); cjv_d = din("c_jv", [128, NE * JMAX_]); cjb_d = din("c_jb", [128, NBLK_ * NE])
    ctk_d = din("c_tokid", [128, NTILE], I32)
    cpk_d = din("c_pk", [128, KC])
    out_d = nc.dram_tensor("out", [NTOK, D], F32, kind="ExternalOutput").ap()
    dk = "ExternalOutput" if debug else "Internal"
    x1_d = nc.dram_tensor("x1s", [NTOK, D], F32, kind=dk).ap()
    h2_d = nc.dram_tensor("h2s", [NTOK + 1, D], BF16, kind=dk).ap()
    slot_d = nc.dram_tensor("slots", [NBLK_ * BLK_, 1], I32, kind=dk).ap()
    Y_d = nc.dram_tensor("ys", [NBLK_ * BLK_, D], F32, kind="Internal").ap()
    wd_d = nc.dram_tensor("wds", [128, NTILE * NE], F32, kind=dk).ap()
    dbg = {}
    if debug:
        for nm, dt_ in (("d_o", F32), ("d_ya", BF16), ("d_yb", BF16), ("d_mg", BF16), ("d_qs", BF16), ("d_it", BF16)):
            dbg[nm] = nc.dram_tensor(nm, [128, H * 512], dt_, kind="ExternalOutput").ap()
    wbf_d = nc.dram_tensor("wbf", [12, 128, KC, 1024], BF16, kind="Internal").ap()
    NSTT = NB * (T // 512)
    hTs_d = nc.dram_tensor("hTs", [NSTT, 128, KC * 512], BF16, kind="Internal").ap()
    its_d = nc.dram_tensor("its", [NSTT, 128, 4 * D], BF16, kind="Internal").ap()
    kes_d = nc.dram_tensor("kes", [NSTT, 128, H * 512], BF16, kind="Internal").ap()
    ebs_d = nc.dram_tensor("ebs", [NSTT, 128, H * 512], BF16, kind="Internal").ap()
    cns_d = nc.dram_tensor("cnss", [NSTT, 128, H * 12], F32, kind="Internal").ap()
    sp_d = nc.dram_tensor("spb", [NB * NCH_B, 128, H * 128], BF16, kind="Internal").ap()

    P = Prog(nc)
    with ExitStack() as es:
        def sb(name, shape, dt=F32):
            return es.enter_context(nc.sbuf_tensor(name, list(shape), dt))

        tm = ExitStack()

        def sbt(name, shape, dt=F32):
            return tm.enter_context(nc.sbuf_tensor(name, list(shape), dt))
        sel = sb("sel", [R, NB * 128]); B_sel = Buf()
        ones_f = sb("ones_f", [128, 128]); ones_b = sb("ones_b", [128, 512], BF16); zeros_f = sb("zeros_f", [128, 128])
        B_const = Buf()
        epsc = sb("epsc", [128, 1]); onec = sb("onec", [128, 1])
        grow = sb("grow", [R, 4, D]); B_grow = Buf()
        fgbc = sb("fgbc", [128, D]); B_fgbc = Buf()
        wdense = sb("wdense", [128, NTILE, NE]); B_wd = [Buf() for _ in range(NTILE)]
        maskall = sb("maskall", [128, NTILE, NE], BF16); B_maskall = Buf()
        identb = sb("identb", [128, 128], BF16); B_identb = Buf()
        B_h2tok = [Buf() for _ in range(NTILE)]; B_h2row = Buf()
        ident = sbt("ident", [128, 128]); B_ident = Buf()
        gcur = sbt("gcur", [128, 4, D]); B_gcur = [Buf() for _ in range(4)]
        maskf = sbt("maskf", [128, 256]); B_mask = Buf()
        vecT = sbt("vecT", [128, 64]); B_vecT = Buf()
        bmodT = sbt("bmodT", [128, 48]); B_bmodT = Buf()
        modT = sbt("modT", [128, 48, R]); B_modT = Buf()
        G1T = sbt("G1T", [128, R, 8]); G2T = sbt("G2T", [128, R, 8]); B_GT = Buf()
        lbs = sbt("lbs", [128, 2, 8]); B_lbs = Buf()
        wsT = sbt("wsT", [128, 8, 128], BF16); B_wsT = Buf()
        bsrow = sbt("bsrow", [1, 8 * 128], BF16); B_bsrow = Buf()
        wr_sb = sbt("wr_sb", [128, KC, NE]); B_wr = Buf()
        brrow = sbt("brrow", [1, NE]); B_br = Buf()
        S = sbt("S", [128, 2, H, 128]); B_S = [[Buf() for _ in range(H)] for _ in range(2)]
        psum = [es.enter_context(nc.psum_tensor("ps%d" % i, [128, 512], F32)) for i in range(8)]
        B_ps = [Buf() for _ in range(8)]
        ps_i = [0]

        def nps():
            i = ps_i[0] % 8
            ps_i[0] += 1
            return psum[i], B_ps[i]

        dsem = {}

        def sem_for(key):
            if key not in dsem:
                dsem[key] = P.new_dma_sem()
            return dsem[key]

        def ACT(out, in_, func, reads, writes, bias=None, scale=None, accum=None, eng="scalar"):
            kw = {}
            if bias is not None:
                kw["bias"] = bias
            if scale is not None:
                kw["scale"] = scale
            if accum is not None:
                kw["accum_out"] = accum
            return P.op(eng, lambda e: e.activation(out=out, in_=in_, func=func, **kw), reads, writes)

        def TS(eng, out, in0, s1, s2, op0, op1, reads, writes):
            if op1 is None:
                return P.op(eng, lambda e: e.tensor_scalar(out=out, in0=in0, scalar1=s1, scalar2=None, op0=op0), reads, writes)
            return P.op(eng, lambda e: e.tensor_scalar(out=out, in0=in0, scalar1=s1, scalar2=s2, op0=op0, op1=op1), reads, writes)

        def TT(eng, out, in0, in1, op, reads, writes):
            return P.op(eng, lambda e: e.tensor_tensor(out=out, in0=in0, in1=in1, op=op), reads, writes)

        def STT(out, in0, scalar, in1, op0, op1, reads, writes, eng="vector"):
            return P.op(eng, lambda e: e.scalar_tensor_tensor(out=out, in0=in0, scalar=scalar, in1=in1, op0=op0, op1=op1), reads, writes)

        def MM(out, lhsT, rhs, start, stop, reads, writes):
            return P.op("tensor", lambda e: e.matmul(out, lhsT, rhs, start=start, stop=stop), reads, writes)

        def TR(out, in_, idn, reads, writes):
            return P.op("tensor", lambda e: e.transpose(out, in_, idn), reads, writes)

        def CP(eng, out, in_, reads, writes):
            if eng == "scalar":
                return P.op(eng, lambda e: e.activation(out=out, in_=in_, func=AF.Copy), reads, writes)
            return P.op(eng, lambda e: e.tensor_copy(out=out, in_=in_), reads, writes)

        def DMA(out, in_, key, reads, writes, eng="sync"):
            return P.dma(eng, lambda e: e.dma_start(out=out, in_=in_), sem_for(key), reads, writes)

        def MEMSET(eng, t, val, writes):
            return P.op(eng, lambda e: e.memset(t, val), (), writes)

        DMA(ident[:], cid_d[:, :], "c0", [], [B_ident])
        DMA(maskf[:], cmk_d[:, :], "c1", [], [B_mask])
        DMA(sel[:], csel_d[0:R, 0:NB * 128], "c2", [], [B_sel])
        MEMSET("vector", ones_f[:], 1.0, [B_const])
        MEMSET("vector", ones_b[:], 1.0, [B_const])
        MEMSET("vector", zeros_f[:], 0.0, [B_const])
        MEMSET("vector", epsc[:], EPS, [B_const])
        MEMSET("vector", onec[:], 1.0, [B_const])
        CP("vector", identb[:], ident[:], [B_ident], [B_identb])
        DMA(fgbc[:], fg_d.rearrange("(o d) -> o d", o=1).partition_broadcast(128), "c3", [], [B_fgbc])
        DMA(brrow[:], br_d[:, :], "c4", [], [B_br])
        DMA(wr_sb[:], wr_d[0].rearrange("(k p) e -> p k e", p=128), "c5", [], [B_wr])

        B_wbf = [Buf() for _ in range(12)]
        with ExitStack() as s0:
            def sb0(name, shape, dt=F32):
                return s0.enter_context(nc.sbuf_tensor(name, list(shape), dt))
            stg = sb0("stg", [64, 128]); B_stg = Buf()
            stg2 = sb0("stg2", [48, 128]); B_stg2 = Buf()
            cc = sb0("cc_sb", [R, D]); B_cc = Buf()
            scc = sb0("scc", [R, D]); B_scc = Buf()
            scT = sb0("scT", [128, KC, R]); B_scT = Buf()
            wm = sb0("wm", [128, KC, 512]); B_wm = Buf()
            bmrow = sb0("bmrow", [1, 6 * D]); B_bmrow = Buf()
            wsf = sb0("wsf", [128, 8, 128]); B_wsf = Buf()
            bsf = sb0("bsf", [1, 8 * 128]); B_bsf = Buf()
            cst = sb0("cst", [128, KC, 1024], BF16); B_cst = [Buf(), Buf()]
            cst2 = sb0("cst2", [128, KC, 1024], BF16)
            csts = [cst, cst2]

            MEMSET("vector", stg[:], 0.0, [B_stg])
            DMA(stg[0:8, :], n1g_d[0].rearrange("(h d) -> h d", d=128), "s0", [], [B_stg])
            DMA(stg[8:16, :], n2g_d[0].rearrange("(h d) -> h d", d=128), "s0", [], [B_stg])
            DMA(stg[16:32, :], lbf_d.rearrange("r (h d) -> (r h) d", d=128), "s0", [], [B_stg])
            DMA(stg[32:48, :], lbb_d.rearrange("r (h d) -> (r h) d", d=128), "s0", [], [B_stg])
            DMA(stg[48:49, :], gng_d[:, :], "s0", [], [B_stg])
            DMA(stg2[:], bmod_d[0].rearrange("(j d) -> j d", d=128), "s1", [], [B_stg2])
            DMA(cc[:], cc_d[:, :], "s2", [], [B_cc])
            DMA(bmrow[:], bmod_d[:, :], "s3", [], [B_bmrow])
            DMA(wsf[:], ws_d[0].rearrange("g t p -> t g p"), "s4", [], [B_wsf])
            DMA(bsf[:], bs_d[0].rearrange("(o g) t -> o (g t)", o=1), "s5", [], [B_bsf])
            CP("vector", bsrow[:], bsf[:], [B_bsf], [B_bsrow])
            ps, bp = nps()
            TR(ps[:, 0:64], stg[:, :], ident[0:64, 0:64], [B_stg, B_ident], [bp])
            CP("vector", vecT[:], ps[:, 0:64], [bp], [B_vecT])
            ps, bp = nps()
            TR(ps[:, 0:48], stg2[:, :], ident[0:48, 0:48], [B_stg2, B_ident], [bp])
            CP("vector", bmodT[:], ps[:, 0:48], [bp], [B_bmodT])
            for g in range(8):
                ps, bp = nps()
                TR(ps[:, 0:128], wsf[:, g, :], ident[:, :], [B_wsf, B_ident], [bp])
                CP("vector", wsT[:, g, :], ps[:, 0:128], [bp], [B_wsT])
            for d_ in range(2):
                o = 16 + 16 * d_
                TT("vector", lbs[:, d_, :], vecT[:, o:o + 8], vecT[:, o + 8:o + 16], ALU.subtract, [B_vecT], [B_lbs])
            ACT(lbs[:], lbs[:], AF.Sigmoid, [B_lbs], [B_lbs])
            ACT(scc[:], cc[:], AF.Sigmoid, [B_cc], [B_scc])
            TT("vector", scc[:], scc[:], cc[:], ALU.mult, [B_scc, B_cc], [B_scc])
            for k in range(KC):
                ps, bp = nps()
                TR(ps[:, 0:R], scc[:, k * 128:(k + 1) * 128], ident[0:R, 0:R], [B_scc, B_ident], [bp])
                CP("vector", scT[:, k, :], ps[:, 0:R], [bp], [B_scT])
            for n in range(12):
                kind = n // 2
                DMA(wm[:], wmod_d[0, :, n * 512:(n + 1) * 512].rearrange("(k p) c -> p k c", p=128), "s6", [], [B_wm])
                if kind in (2, 3, 4, 5):
                    ps, bp = nps()
                    for k in range(KC):
                        MM(ps[0:R, :], scT[:, k, :], wm[:, k, :], k == 0, False, [B_scT, B_wm], [bp])
                    MM(ps[0:R, :], ones_f[0:1, 0:R], bmrow[0:1, n * 512:(n + 1) * 512], False, True, [B_const, B_bmrow], [bp])
                    CP("vector", grow[:, {2: 0, 5: 1, 3: 2, 4: 3}[kind], (n % 2) * 512:(n % 2) * 512 + 512], ps[0:R, :], [bp], [B_grow])
                if kind in (0, 1, 3, 4):
                    for jj in range(4):
                        j = n * 4 + jj
                        ps, bp = nps()
                        for k in range(KC):
                            MM(ps[:, 0:R], wm[:, k, jj * 128:(jj + 1) * 128], scT[:, k, :], k == 0, k == KC - 1, [B_scT, B_wm], [bp])
                        ACT(modT[:, j, :], ps[:, 0:R], AF.Identity, [bp, B_bmodT], [B_modT], bias=bmodT[:, j:j + 1])
            for r in range(R):
                for (GT, kind, vo) in ((G1T, 1, 0), (G2T, 4, 8)):
                    STT(GT[:, r, :], modT[:, kind * 8:kind * 8 + 8, r], 1.0, vecT[:, vo:vo + 8], ALU.add, ALU.mult, [B_modT, B_vecT], [B_GT])
            n2gR = sb0("n2gR", [R, D]); B_n2gR = Buf()
            DMA(n2gR[:], n2g_d[0:1, :].partition_broadcast(R).rearrange("p o d -> p (o d)"), "s7", [], [B_n2gR])
            STT(grow[:, 3, :], grow[:, 3, :], 1.0, n2gR[:, :], ALU.add, ALU.mult, [B_grow, B_n2gR], [B_grow])
            srcs = [win_d[0, :, g * 1024:(g + 1) * 1024] for g in range(9)] + [wba_d[0], wbb_d[0], wout_d[0]]
            for g, src in enumerate(srcs):
                c_ = csts[g % 2]
                DMA(c_[:], src.rearrange("(k p) n -> p k n", p=128), "cst%d" % (g % 2), [], [B_cst[g % 2]], eng="gpsimd")
                DMA(wbf_d[g], c_[:], "cstw%d" % (g % 2), [B_cst[g % 2]], [B_wbf[g]])
        P.barrier()
        with tm:
            xt = sbt("xt", [128, 4, D]); B_xt = [Buf() for _ in range(4)]
            stat = sbt("stat", [128, 16]); B_stat = Buf()
            hT = sbt("hT", [128, KC, ST], BF16); B_hT = [Buf() for _ in range(KC)]
            wsl = [sbt("wsl0", [128, KC, 1024], BF16), sbt("wsl1", [128, KC, 1024], BF16)]
            B_wsl = [Buf(), Buf()]
            qs = sbt("qs", [128, H, ST], BF16); B_qs = [Buf() for _ in range(H)]
            itok = sbt("itok", [128, 4, D], BF16); B_itok = [Buf() for _ in range(4)]
            qe = sbt("qe", [128, H, ST], BF16); B_qe = [Buf() for _ in range(H)]
            keT = sbt("keT", [128, H, ST], BF16); B_keT = [Buf() for _ in range(H)]
            ketok1 = sbt("ketok", [128, D], BF16); B_ketok1 = Buf()
            cns = sbt("cns", [128, H, 3, 4]); B_cns = [Buf() for _ in range(H)]
            sog = sbt("sog", [128, H, ST], BF16); B_sog = [Buf() for _ in range(H)]
            xtb = xt[:].rearrange("p c d -> p (c d)").bitcast(BF16)
            gu = xtb[:, 0:H * ST].rearrange("p (h t) -> p h t", t=ST); B_gu = [B_xt[0]] * 4 + [B_xt[1]] * 4
            vn = xtb[:, H * ST:2 * H * ST].rearrange("p (c d) -> p c d", d=D); B_vn = [B_xt[2], B_xt[2], B_xt[3], B_xt[3]]
            oT = sbt("oT", [128, H, ST]); B_oT = [Buf() for _ in range(H)]
            yaT = keT; B_yaT = B_keT
            ybT = qe; B_ybT = B_qe
            Sp = sbt("Sp", [128, H, 128], BF16); B_Sp = [Buf() for _ in range(H)]
            gv = sbt("gv", [128, D]); B_gv = Buf()
            xr = gv; B_xr = B_gv; junk = gv; B_junk = B_gv
            bst = sbt("bst", [128, 16]); B_bst = Buf()
            h2tok = sbt("h2tok", [128, D], BF16); B_h2t = Buf()
            rt = sbt("rt", [128, 4, NE]); B_rt = Buf()
            top8 = sbt("top8", [128, 16]); B_top8 = Buf()
            tmps = [sbt("tmp%d" % i, [128, ST]) for i in range(6)]; B_tmps = [Buf() for _ in range(6)]
            sTs = [sbt("sT%d" % i, [128, 4, 128], BF16) for i in range(2)]; B_sTs = [Buf(), Buf()]
            B_spd = [Buf() for _ in range(NB * NCH_B)]
            B_x1d = [Buf() for _ in range(NTILE)]
            B_scr = [[Buf() for _ in range(5)] for _ in range(NSTT)]
            tmp_i = [0]; st_i = [0]; w_i = [0]

            tmp_pool = [[(tmps[i], B_tmps[i]) for i in range(6)]]
            base_pool = tmp_pool[0]

            def ntmp():
                pool = tmp_pool[0]
                i = tmp_i[0] % len(pool)
                tmp_i[0] += 1
                return pool[i]

            def nst():
                i = st_i[0] % 2
                st_i[0] += 1
                return sTs[i], B_sTs[i]

            def wload(g):
                s_ = w_i[0] % 2
                w_i[0] += 1
                DMA(wsl[s_][:], wbf_d[g], "wsl%d" % s_, [B_wbf[g]], [B_wsl[s_]])
                return wsl[s_], B_wsl[s_]

            def v3(ap_, t=128):
                return ap_.rearrange("p (c t) -> p c t", t=t)

            def norm_T(r, nch, GT, shkind, outs):
                NT = nch * 128
                for c in range(nch):
                    ACT(junk[:], xt[:, c, :], AF.Square, [B_xt[c]], [B_junk, B_stat], accum=stat[:, c:c + 1])
                ACT(stat[:, 4:4 + nch], stat[:, 0:nch], AF.Ln, [B_stat], [B_stat], scale=1.0 / D, bias=epsc[:, 0:1])
                ACT(stat[:, 8:8 + nch], stat[:, 4:4 + nch], AF.Exp, [B_stat], [B_stat], scale=-0.5)
                for c in range(nch):
                    TS("vector", xt[:, c, :], xt[:, c, :], stat[:, 8 + c:9 + c], None, ALU.mult, None, [B_xt[c], B_stat], [B_xt[c]])
                for k in range(KC):
                    ps, bp = nps()
                    for c in range(nch):
                        TR(ps[:, c * 128:(c + 1) * 128], xt[:, c, k * 128:(k + 1) * 128], ident[:, :], [B_xt[c], B_ident], [bp])
                    for (ot, ob) in outs:
                        ACT(ot[:, k, 0:NT], ps[:, 0:NT], AF.Identity, [bp, B_GT, B_modT], [ob[k]],
                            scale=GT[:, r, k:k + 1], bias=modT[:, shkind * 8 + k, r:r + 1])

            def proj_fm(wt, bw, h, NT, src=None, bsrc=None):
                src = hT if src is None else src
                bsrc = B_hT if bsrc is None else bsrc
                ps, bp = nps()
                for k in range(KC):
                    MM(ps[:, 0:NT], wt[:, k, h * 128:(h + 1) * 128], src[:, k, 0:NT], k == 0, k == KC - 1, [bw, bsrc[k]], [bp])
                return ps, bp

            def proj_tm(wt, bw, c, n, src=None, bsrc=None):
                src = hT if src is None else src
                bsrc = B_hT if bsrc is None else bsrc
                ps, bp = nps()
                for k in range(KC):
                    MM(ps[:, :], src[:, k, c * 128:(c + 1) * 128], wt[:, k, n * 512:(n + 1) * 512], k == 0, k == KC - 1, [bw, bsrc[k]], [bp])
                return ps, bp

            def decay_prep(dr, h, ps, bp, nch, need_q):
                NT = nch * 128
                e_, be = ntmp(); l1, b1 = ntmp(); l2, b2 = ntmp()
                ACT(e_[:, 0:NT], ps[:, 0:NT], AF.Exp, [bp], [be], scale=-1.0)
                ACT(l1[:, 0:NT], e_[:, 0:NT], AF.Ln, [be], [b1], bias=onec[:, 0:1])
                ACT(l2[:, 0:NT], e_[:, 0:NT], AF.Ln, [be, B_lbs], [b2], scale=lbs[:, dr, h:h + 1], bias=onec[:, 0:1])
                yield
                TT("vector", l2[:, 0:NT], l2[:, 0:NT], l1[:, 0:NT], ALU.subtract, [b2, b1], [b2])
                ACT(l1[:, 0:NT], l2[:, 0:NT], AF.Exp, [b2], [b1])
                TS("gpsimd", l1[:, 0:NT], l1[:, 0:NT], -1.0, 1.0, ALU.mult, ALU.add, [b1], [b1])
                yield
                for c in range(nch):
                    cs = slice(c * 128, (c + 1) * 128)
                    P.op("vector", lambda e, cs=cs: e.tensor_tensor_scan(out=e_[:, cs], data0=l2[:, cs], data1=zeros_f[:, 0:128],
                                                                          initial=0.0, op0=ALU.add, op1=ALU.add), [b2, B_const], [be])
                yield
                P63b = v3(e_[:, 0:NT])[:, :, 63:64].to_broadcast([128, nch, 128])
                P63v = e_[:, 63:NT:128]; P127v = e_[:, 127:NT:128]
                if dr == 0:
                    TT("vector", v3(l2[:, 0:NT]), v3(e_[:, 0:NT]), P63b, ALU.subtract, [be], [b2])
                else:
                    TT("vector", l2[:, 0:NT], l2[:, 0:NT], e_[:, 0:NT], ALU.subtract, [be, b2], [b2])
                    TT("vector", v3(l2[:, 0:NT]), v3(l2[:, 0:NT]), P63b, ALU.add, [be, b2], [b2])
                ia, ib = (0, 1) if dr == 0 else (1, 0)
                ACT(cns[:, h, ia, 0:nch], P63v, AF.Exp, [be], [B_cns[h]])
                TT("vector", cns[:, h, ib, 0:nch], P127v, P63v, ALU.subtract, [be], [B_cns[h]])
                ACT(cns[:, h, ib, 0:nch], cns[:, h, ib, 0:nch], AF.Exp, [B_cns[h]], [B_cns[h]])
                ACT(cns[:, h, 2, 0:nch], P127v, AF.Exp, [be], [B_cns[h]])
                yield
                E_, bE = e_, be
                if need_q == "q":
                    ACT(E_[:, 0:NT], l2[:, 0:NT], AF.Exp, [b2], [bE])
                    TT("vector", qe[:, h, 0:NT], qs[:, h, 0:NT], E_[:, 0:NT], ALU.mult, [B_qs[h], bE], [B_qe[h]])
                elif need_q == "E":
                    ACT(qe[:, h, 0:NT], l2[:, 0:NT], AF.Exp, [b2], [B_qe[h]])
                ACT(E_[:, 0:NT], l2[:, 0:NT], AF.Exp, [b2], [bE], scale=-1.0)
                TT("gpsimd", keT[:, h, 0:NT], l1[:, 0:NT], E_[:, 0:NT], ALU.mult, [b1, bE], [B_keT[h]])

            def gla(dr, mode, nch, b, chunk0):
                order = range(nch) if dr == 0 else range(nch - 1, -1, -1)
                upd = (dr == 0) or (mode != "main")
                outp = mode == "main"
                for c in order:
                    cs = slice(c * 128, (c + 1) * 128)
                    cg = b * NCH_B + chunk0 + c
                    if upd:
                        ps, bp = nps()
                        psb = ps[:].bitcast(BF16)
                        for h in range(H):
                            TR(psb[:, h * 128:(h + 1) * 128], keT[:, h, cs], identb[:, :], [B_keT[h], B_identb], [bp])
                        CP("scalar", ketok1[:, :], psb[:, :], [bp], [B_ketok1])
                    if outp and dr == 1:
                        DMA(Sp[:].rearrange("p h v -> p (h v)"), sp_d[cg], "spl", [B_spd[cg]], B_Sp)
                    elif outp or mode == "pre":
                        for h in range(H):
                            if h % 2:
                                ACT(Sp[:, h, :], S[:, dr, h, :], AF.Copy, [B_S[dr][h], B_cns[h]], [B_Sp[h]], scale=cns[:, h, 0, c:c + 1])
                            else:
                                TS("vector", Sp[:, h, :], S[:, dr, h, :], cns[:, h, 0, c:c + 1], None, ALU.mult, None,
                                   [B_S[dr][h], B_cns[h]], [B_Sp[h]])
                        if mode == "pre":
                            DMA(sp_d[cg], Sp[:].rearrange("p h v -> p (h v)"), "sps", B_Sp, [B_spd[cg]])
                    if outp:
                        for hg in range(2):
                            hs = range(hg * 4, hg * 4 + 4)
                            ps, bp = nps()
                            for hh, h in enumerate(hs):
                                MM(ps[:, hh * 128:(hh + 1) * 128], keT[:, h, cs], qe[:, h, cs], True, True, [B_keT[h], B_qe[h]], [bp])
                            st_, bst_ = nst()
                            TT("vector", st_[:, :, :], v3(ps[:, :]), maskf[:, dr * 128:(dr + 1) * 128].unsqueeze(1).to_broadcast([128, 4, 128]), ALU.mult, [bp, B_mask], [bst_])
                            ps2, bp2 = nps()
                            for hh, h in enumerate(hs):
                                MM(ps2[:, hh * 128:(hh + 1) * 128], itok[:, c, h * 128:(h + 1) * 128], st_[:, hh, :], True, False, [B_itok[c], bst_], [bp2])
                                MM(ps2[:, hh * 128:(hh + 1) * 128], Sp[:, h, :], qe[:, h, cs], False, True, [B_Sp[h], B_qe[h]], [bp2])
                            bo = [B_oT[h] for h in hs]
                            if dr == 0:
                                CP("scalar", oT[:, hg * 4:hg * 4 + 4, cs], v3(ps2[:, :]), [bp2], bo)
                            else:
                                TT("vector", oT[:, hg * 4:hg * 4 + 4, cs], oT[:, hg * 4:hg * 4 + 4, cs], v3(ps2[:, :]), ALU.add, [bp2] + bo, bo)
                    if upd:
                        for hg in range(2):
                            ps, bp = nps()
                            for hh in range(4):
                                h = hg * 4 + hh
                                MM(ps[:, hh * 128:(hh + 1) * 128], ketok1[:, h * 128:(h + 1) * 128], itok[:, c, h * 128:(h + 1) * 128], True, True,
                                   [B_ketok1, B_itok[c]], [bp])
                            tu, btu = ntmp()
                            for hh in range(4):
                                h = hg * 4 + hh
                                ACT(tu[:, hh * 128:(hh + 1) * 128], ps[:, hh * 128:(hh + 1) * 128], AF.Copy, [bp, B_cns[h]], [btu], scale=cns[:, h, 1, c:c + 1])
                            for hh in range(4):
                                h = hg * 4 + hh
                                STT(S[:, dr, h, :], S[:, dr, h, :], cns[:, h, 2, c:c + 1], tu[:, hh * 128:(hh + 1) * 128], ALU.mult, ALU.add,
                                    [btu, B_cns[h], B_S[dr][h]], [B_S[dr][h]])
                    yield

            def interleave(gens):
                gens = list(gens)
                while gens:
                    for g_ in list(gens):
                        try:
                            next(g_)
                        except StopIteration:
                            gens.remove(g_)

            preloaded = [-1]

            def st_pass(mode, b, r, src, nch, chunk0, nxt=None):
                NT = nch * 128
                sidx_ = b * NST + chunk0 // 4
                if mode == "main":
                    if preloaded[0] != sidx_:
                        DMA(hT[:].rearrange("p k t -> p (k t)"), hTs_d[sidx_], "hTl", [B_scr[sidx_][0]], B_hT)
                        DMA(itok[:].rearrange("p c d -> p (c d)"), its_d[sidx_], "itl", [B_scr[sidx_][1]], B_itok)
                else:
                    DMA(xt[:, 0:nch, :], src.rearrange("(c p) d -> p c d", p=128), "xt", [], B_xt[0:nch])
                    norm_T(r, nch, G1T, 0, [(hT, B_hT)])
                    if mode == "pre":
                        DMA(hTs_d[sidx_], hT[:].rearrange("p k t -> p (k t)"), "hTs", B_hT, [B_scr[sidx_][0]])
                if mode == "main":
                    glist = [0, 2, 4, 5, 6, 9, 7, 10, 8, 11]
                elif mode == "pre":
                    glist = [1, 3]
                else:
                    glist = [1, 2, 3]
                pend = [wload(glist[0])]
                gi = [0]

                def wpre():
                    if gi[0] < len(glist):
                        pend[0] = wload(glist[gi[0]])

                def wnext(prefetch=True):
                    cur = pend[0]
                    gi[0] += 1
                    if prefetch:
                        wpre()
                    return cur

                if mode == "main":
                    wt, bw = wnext()
                    for h in range(H):
                        ps, bp = proj_fm(wt, bw, h, NT)
                        ACT(qs[:, h, 0:NT], ps[:, 0:NT], AF.Silu, [bp], [B_qs[h]])
                def i_gen(wt_, bw_):
                    for c in range(nch):
                        for n in range(2):
                            ps, bp = proj_tm(wt_, bw_, c, n)
                            CP("scalar" if n else "vector", itok[:, c, n * 512:(n + 1) * 512], ps[:, :], [bp], [B_itok[c]])
                            yield
                    if mode == "pre":
                        DMA(its_d[sidx_], itok[:].rearrange("p c d -> p (c d)"), "its", B_itok, [B_scr[sidx_][1]])

                def fm_act(wt_, bw_, dst, bdst, func, per=2):
                    for h_ in range(H):
                        ps_, bp_ = proj_fm(wt_, bw_, h_, NT)
                        ACT(dst[:, h_, 0:NT], ps_[:, 0:NT], func, [bp_], [bdst[h_]])
                        if h_ % per == per - 1:
                            yield

                def decay_all(dr_, wt_, bw_, extra_gens=()):
                    if mode == "main":
                        extra = [(xt[:, c_, hf * 512:(hf + 1) * 512], B_xt[c_]) for c_ in range(4) for hf in range(2)]
                    else:
                        extra = [(oT[:, h_, :], B_oT[h_]) for h_ in range(H)]
                    tmp_pool[0] = base_pool + extra
                    for h0 in range(0, H, 4):
                        gs = []
                        for h_ in range(h0, h0 + 4):
                            ps_, bp_ = proj_fm(wt_, bw_, h_, NT)
                            gs.append(decay_prep(dr_, h_, ps_, bp_, nch, "q" if mode == "main" else ("E" if mode == "pre" else None)))
                        interleave(gs + list(extra_gens))
                        extra_gens = ()
                    tmp_pool[0] = base_pool

                if mode != "main":
                    wI, bI = wnext()
                    ig = i_gen(wI, bI)
                    for di, dr in enumerate((0, 1) if mode != "pre" else (1,)):
                        wt, bw = wnext(False)
                        decay_all(dr, wt, bw, [ig] if di == 0 else [])
                        if di == 0:
                            interleave([ig])
                        wpre()
                        if mode == "pre":
                            DMA(kes_d[sidx_], keT[:].rearrange("p h t -> p (h t)"), "kes", B_keT, [B_scr[sidx_][2]])
                            DMA(ebs_d[sidx_], qe[:].rearrange("p h t -> p (h t)"), "ebs", B_qe, [B_scr[sidx_][3]])
                            DMA(cns_d[sidx_], cns[:].rearrange("p h a c -> p (h a c)"), "cnss", B_cns, [B_scr[sidx_][4]])
                        interleave([gla(dr, mode, nch, b, chunk0)])
                    return
                wt, bw = wnext()
                decay_all(0, wt, bw)
                wt, bw = wnext()
                interleave([gla(0, mode, nch, b, chunk0), fm_act(wt, bw, sog, B_sog, AF.Silu)])
                DMA(keT[:].rearrange("p h t -> p (h t)"), kes_d[sidx_], "kel", [B_scr[sidx_][2]], B_keT)
                DMA(qe[:].rearrange("p h t -> p (h t)"), ebs_d[sidx_], "ebl", [B_scr[sidx_][3]], B_qe)
                DMA(cns[:].rearrange("p h a c -> p (h a c)"), cns_d[sidx_], "cnl", [B_scr[sidx_][4]], B_cns)
                for h in range(H):
                    TT("gpsimd" if h % 2 else "vector", qe[:, h, 0:NT], qe[:, h, 0:NT], qs[:, h, 0:NT], ALU.mult, [B_qe[h], B_qs[h]], [B_qe[h]])
                wU, bU = wnext(); wV, bV = wnext(False)

                def v_gen():
                    for c in range(nch):
                        for n in range(2):
                            ps, bp = proj_tm(wV, bV, c, n)
                            ACT(gv[:, n * 512:(n + 1) * 512], ps[:, :], AF.Gelu, [bp], [B_gv])
                            P.op("vector", lambda e, n=n: e.bn_stats(out=bst[:, n * 6:(n + 1) * 6], in_=gv[:, n * 512:(n + 1) * 512]), [B_gv], [B_bst])
                        P.op("vector", lambda e: e.bn_aggr(out=bst[:, 12:14], in_=bst[:, 0:12]), [B_bst], [B_bst])
                        ACT(bst[:, 14:15], bst[:, 13:14], AF.Ln, [B_bst], [B_bst], bias=epsc[:, 0:1])
                        ACT(bst[:, 15:16], bst[:, 14:15], AF.Exp, [B_bst], [B_bst], scale=-0.5)
                        TS("vector", vn[:, c, :], gv[:, :], bst[:, 12:13], bst[:, 15:16], ALU.subtract, ALU.mult, [B_gv, B_bst], [B_vn[c]])
                        yield

                interleave([gla(1, mode, nch, b, chunk0), fm_act(wU, bU, gu, B_gu, AF.Gelu), v_gen()])
                wpre()
                for h in range(H):
                    sq, bsq = ntmp()
                    TT("gpsimd", sq[:, 0:NT], oT[:, h, 0:NT], oT[:, h, 0:NT], ALU.mult, [B_oT[h]], [bsq])
                    ps, bp = nps()
                    MM(ps[:, 0:NT], ones_f[:, 0:128], sq[:, 0:NT], True, True, [B_const, bsq], [bp])
                    l_, bl = ntmp()
                    ACT(l_[:, 0:NT], ps[:, 0:NT], AF.Ln, [bp], [bl], scale=1.0 / 128, bias=epsc[:, 0:1])
                    ACT(l_[:, 0:NT], l_[:, 0:NT], AF.Exp, [bl], [bl], scale=-0.5)
                    TT("vector", l_[:, 0:NT], l_[:, 0:NT], oT[:, h, 0:NT], ALU.mult, [bl, B_oT[h]], [bl])
                    STT(yaT[:, h, 0:NT], l_[:, 0:NT], vecT[:, 48:49], sog[:, h, 0:NT], ALU.mult, ALU.mult, [bl, B_vecT, B_sog[h]], [B_yaT[h]])
                for c in range(nch):
                    cs = slice(c * 128, (c + 1) * 128)
                    for hg in range(2):
                        ps, bp = nps()
                        for hh in range(4):
                            g = hg * 4 + hh
                            MM(ps[:, hh * 128:(hh + 1) * 128], vn[:, c, g * 128:(g + 1) * 128], wsT[:, g, :], True, False, [B_vn[c], B_wsT], [bp])
                            MM(ps[:, hh * 128:(hh + 1) * 128], ones_b[0:1, 0:128], bsrow[0:1, g * 128:(g + 1) * 128], False, True, [B_const, B_bsrow], [bp])
                        bo = [B_ybT[g] for g in range(hg * 4, hg * 4 + 4)]
                        TT("vector", ybT[:, hg * 4:hg * 4 + 4, cs], gu[:, hg * 4:hg * 4 + 4, cs], v3(ps[:, :]), ALU.mult,
                           [bp] + [B_gu[g] for g in range(hg * 4, hg * 4 + 4)], bo)
                if debug and b == 0 and chunk0 == 0:
                    DMA(dbg["d_ya"][:, :], yaT[:].rearrange("p h t -> p (h t)"), "dbg", B_yaT, [Buf()])
                    DMA(dbg["d_yb"][:, :], ybT[:].rearrange("p h t -> p (h t)"), "dbg", B_ybT, [Buf()])
                wA, bA = wnext(); wG, bG = wnext(False)
                for j in range(H):
                    psA, bpA = proj_fm(wA, bA, j, NT, yaT, B_yaT)
                    psG, bpG = proj_fm(wG, bG, j, NT)
                    sg, bsg = ntmp()
                    ACT(sg[:, 0:NT], psG[:, 0:NT], AF.Sigmoid, [bpG], [bsg])
                    TT("vector", oT[:, j, 0:NT], sg[:, 0:NT], psA[:, 0:NT], ALU.mult, [bsg, bpA], [B_oT[j]])
                wpre()
                wB, bB = wnext(); wG, bG = wnext(False)
                for j in range(H):
                    psB, bpB = proj_fm(wB, bB, j, NT, ybT, B_ybT)
                    psG, bpG = proj_fm(wG, bG, j, NT)
                    sg, bsg = ntmp()
                    ACT(sg[:, 0:NT], psG[:, 0:NT], AF.Sigmoid, [bpG], [bsg])
                    TT("vector", sg[:, 0:NT], sg[:, 0:NT], psB[:, 0:NT], ALU.mult, [bsg, bpB], [bsg])
                    TT("gpsimd", qs[:, j, 0:NT], sg[:, 0:NT], oT[:, j, 0:NT], ALU.add, [bsg, B_oT[j]], [B_qs[j]])
                if debug and b == 0 and chunk0 == 0:
                    DMA(dbg["d_mg"][:, :], qs[:].rearrange("p h t -> p (h t)"), "dbg", B_qs, [Buf()])
                if nxt is not None:
                    DMA(hT[:].rearrange("p k t -> p (k t)"), hTs_d[nxt], "hTl", [B_scr[nxt][0]], B_hT)
                    DMA(itok[:].rearrange("p c d -> p (c d)"), its_d[nxt], "itl", [B_scr[nxt][1]], B_itok)
                    preloaded[0] = nxt
                wpre()
                wO, bO = wnext()
                for c in range(nch):
                    tg = (b * T) // 128 + chunk0 + c
                    DMA(xr[:], src[c * 128:(c + 1) * 128, :], "xr", [], [B_xr])
                    for n in range(2):
                        ps, bp = proj_tm(wO, bO, c, n, qs, B_qs)
                        t_, bt = ntmp()
                        TT("vector", t_[:, :], ps[:, :], gcur[:, 0, n * 512:(n + 1) * 512], ALU.mult, [bp, B_gcur[0]], [bt])
                        TT("vector", xt[:, c, n * 512:(n + 1) * 512], t_[:, :], xr[:, n * 512:(n + 1) * 512], ALU.add, [bt, B_xr], [B_xt[c]])
                    DMA(x1_d[tg * 128:(tg + 1) * 128, :], xt[:, c, :], "x1s", [B_xt[c]], [B_x1d[tg]])
                norm_T(b, nch, G2T, 3, [(oT, B_oT)])
                for c in range(nch):
                    tg = (b * T) // 128 + chunk0 + c
                    TT("vector", gv[:, :], xt[:, c, :], gcur[:, 3, :], ALU.mult, [B_xt[c], B_gcur[3]], [B_gv])
                    TT("vector", h2tok[:, :], gv[:, :], gcur[:, 2, :], ALU.add, [B_gv, B_gcur[2]], [B_h2t])
                    DMA(h2_d[tg * 128:(tg + 1) * 128, :], h2tok[:, :], "h2s", [B_h2t], [B_h2tok[tg]])
                for c in range(nch):
                    cs = slice(c * 128, (c + 1) * 128)
                    tg = (b * T) // 128 + chunk0 + c
                    ps, bp = nps()
                    for k in range(KC):
                        MM(ps[:, 0:NE], oT[:, k, cs], wr_sb[:, k, :], k == 0, False, [B_oT[k], B_wr], [bp])
                    MM(ps[:, 0:NE], ones_f[0:1, 0:128], brrow[0:1, :], False, True, [B_const, B_br], [bp])
                    lg = rt[:, 0, :]; mk = rt[:, 1, :]; ex = rt[:, 2, :]
                    CP("vector", lg, ps[:, 0:NE], [bp], [B_rt])
                    P.op("vector", lambda e, lg=lg: e.max(out=top8[:, 0:8], in_=lg), [B_rt], [B_top8])
                    TS("vector", mk, lg, top8[:, 3:4], None, ALU.is_ge, None, [B_rt, B_top8], [B_rt])
                    CP("gpsimd", maskall[:, tg, :], mk, [B_rt], [B_maskall])
                    TS("vector", top8[:, 8:9], top8[:, 0:1], -1.0, None, ALU.mult, None, [B_top8], [B_top8])
                    ACT(ex, lg, AF.Exp, [B_rt, B_top8], [B_rt], bias=top8[:, 8:9])
                    TT("vector", ex, ex, mk, ALU.mult, [B_rt], [B_rt])
                    P.op("vector", lambda e, ex=ex: e.reduce_sum(out=top8[:, 9:10], in_=ex, axis=mybir.AxisListType.X), [B_rt], [B_top8])
                    P.op("vector", lambda e: e.reciprocal(out=top8[:, 10:11], in_=top8[:, 9:10]), [B_top8], [B_top8])
                    TS("vector", wdense[:, tg, :], ex, top8[:, 10:11], None, ALU.mult, None, [B_rt, B_top8], [B_wd[tg]])

            def bcast_g(b, which):
                for n in range(2):
                    ps, bp = nps()
                    MM(ps[:, :], sel[0:R, b * 128:(b + 1) * 128], grow[:, which, n * 512:(n + 1) * 512], True, True, [B_sel, B_grow], [bp])
                    CP("scalar", gcur[:, which, n * 512:(n + 1) * 512], ps[:, :], [bp], [B_gcur[which]])

            for b in range(NB):
                for dr in range(2):
                    for h in range(H):
                        MEMSET("gpsimd", S[:, dr, h, :], 0.0, [B_S[dr][h]])
                bcast_g(b, 0); bcast_g(b, 2); bcast_g(b, 3)
                st_pass("ctx", b, NB, ctx_d[b], CTXL // 128, 0)
                for sti in range(NST - 1, -1, -1):
                    st_pass("pre", b, b, x_d[b, sti * ST:(sti + 1) * ST, :], 4, sti * 4)
                for sti in range(NST):
                    st_pass("main", b, b, x_d[b, sti * ST:(sti + 1) * ST, :], 4, sti * 4,
                            nxt=(b * NST + sti + 1) if sti + 1 < NST else None)
            if debug:
                DMA(wd_d[:, :], wdense[:].rearrange("p t e -> p (t e)"), "wds", B_wd, [Buf()])

        P.barrier()
        BLK = 512
        NBLK = (4 * NTOK) // BLK + NE
        CAP = NBLK * BLK
        JMAX = NTOK // BLK if NTOK >= BLK else 1
        vals = sb("vals", [128, NTILE, NE]); B_vals = Buf()
        dk = sb("dk", [128, NTILE, 8]); B_dk = Buf()
        idx = sb("idx", [128, NTILE, 4], I32); B_idx = Buf()
        be_f = sb("be_f", [128, NBLK]); be1k = sb("be1k", [128, NBLK]); be16 = sb("be16", [128, NBLK]); B_be = Buf()
        pkc = sb("pkc", [128, KC]); B_pkc = Buf()
        DMA(pkc[:], cpk_d[:, :], "c7", [], [B_pkc])
        tokid = sb("tokid", [128, NTILE], I32); B_tokid = Buf()
        DMA(tokid[:], ctk_d[:, :], "c6", [], [B_tokid])
        B_slot = Buf()
        with ExitStack() as ra:
            def sbr(name, shape, dt=F32):
                return ra.enter_context(nc.sbuf_tensor(name, list(shape), dt))
            lst = sbr("lst", [128, 128]); lstb = sbr("lstb", [128, 128], BF16); B_lst = Buf()
            cnt_all = sbr("cnt_all", [128, NTILE, NE]); B_cnt = Buf()
            base_all = sbr("base_all", [128, NTILE, NE]); B_base = Buf()
            jv = sbr("jv", [128, NE, JMAX]); B_jv = Buf()
            jb = sbr("jb", [128, NBLK, NE]); B_jb = Buf()
            cmpj = sbr("cmpj", [128, NBLK, NE]); B_cmpj = Buf()
            sm = sbr("sm", [128, 8, NE]); B_sm = Buf()
            sent = sbr("sent", [128, CAP // 128], I32); sentf = sbr("sentf", [128, CAP // 128]); B_sent = Buf()
            bef = sbr("bef", [128, NBLK]); B_bef = Buf()
            DMA(lst[:], clst_d[:, :], "r0", [], [B_lst])
            CP("vector", lstb[:], lst[:], [B_lst], [B_lst])
            DMA(jv[:], cjv_d[:, :].rearrange("p (e j) -> p e j", j=JMAX), "r1", [], [B_jv])
            DMA(jb[:], cjb_d[:, :].rearrange("p (j e) -> p j e", e=NE), "r2", [], [B_jb])
            MEMSET("vector", sentf[:], float(NTOK), [B_sent])
            CP("vector", sent[:], sentf[:], [B_sent], [B_sent])
            DMA(slot_d.rearrange("(p q) o -> p (q o)", p=128), sent[:], "r3", [B_sent], [B_slot])
            zrow = sbr("zrow", [1, 512]); B_zrow = Buf()
            MEMSET("vector", zrow[:], 0.0, [B_zrow])
            DMA(h2_d[NTOK:NTOK + 1, :], zrow[0:1, :].bitcast(BF16), "r4", [B_zrow], [B_h2row])
            GT_ = 16
            for g0 in range(0, NTILE, GT_):
                gn = min(GT_, NTILE - g0)
                rhs = maskall[:, g0:g0 + gn, :].rearrange("p n e -> p (n e)")
                ps, bp = nps()
                MM(ps[:, 0:gn * NE], lstb[:, :], rhs, True, True, [B_lst, B_maskall], [bp])
                CP("vector", vals[:, g0:g0 + gn, :].rearrange("p n e -> p (n e)"), ps[:, 0:gn * NE], [bp], [B_vals])
                ps2, bp2 = nps()
                MM(ps2[:, 0:gn * NE], ones_b[:, 0:128], rhs, True, True, [B_const, B_maskall], [bp2])
                CP("scalar", cnt_all[:, g0:g0 + gn, :].rearrange("p n e -> p (n e)"), ps2[:, 0:gn * NE], [bp2], [B_cnt])
            MEMSET("vector", base_all[:, 0, :], 0.0, [B_base])
            for n in range(1, NTILE):
                TT("vector", base_all[:, n, :], base_all[:, n - 1, :], cnt_all[:, n - 1, :], ALU.add, [B_base, B_cnt], [B_base])
            tot = sm[:, 0, :]; nbk = sm[:, 1, :]; pend_ = sm[:, 2, :]; pst = sm[:, 3, :]
            TT("vector", tot, base_all[:, NTILE - 1, :], cnt_all[:, NTILE - 1, :], ALU.add, [B_base, B_cnt], [B_sm])
            TT("vector", cmpj[:, 0:NE, 0:JMAX], tot.unsqueeze(2).to_broadcast([128, NE, JMAX]), jv[:, :, :], ALU.is_gt, [B_sm, B_jv], [B_cmpj])
            P.op("vector", lambda e: e.reduce_sum(out=nbk, in_=cmpj[:, 0:NE, 0:JMAX], axis=mybir.AxisListType.X), [B_cmpj], [B_sm])
            TS("vector", nbk, nbk, float(BLK), None, ALU.mult, None, [B_sm], [B_sm])
            P.op("vector", lambda e: e.tensor_tensor_scan(out=pend_, data0=nbk, data1=zeros_f[:, 0:NE], initial=0.0, op0=ALU.add, op1=ALU.add),
                 [B_sm, B_const], [B_sm])
            TT("vector", pst, pend_, nbk, ALU.subtract, [B_sm], [B_sm])
            TT("vector", vals[:, :, :], vals[:, :, :], base_all[:, :, :], ALU.add, [B_vals, B_base], [B_vals])
            TT("vector", vals[:, :, :], vals[:, :, :], pst.unsqueeze(1).to_broadcast([128, NTILE, NE]), ALU.add, [B_vals, B_sm], [B_vals])
            STT(vals[:, :, :], vals[:, :, :], 1.0, maskall[:, :, :], ALU.add, ALU.mult, [B_vals, B_maskall], [B_vals])
            for n in range(NTILE):
                P.op("vector", lambda e, n=n: e.max(out=dk[:, n, :], in_=vals[:, n, :]), [B_vals], [B_dk])
            TS("vector", idx[:, :, :], dk[:, :, 0:4], -1.0, None, ALU.add, None, [B_dk], [B_idx])
            TT("vector", cmpj[:, :, :], pend_.unsqueeze(1).to_broadcast([128, NBLK, NE]), jb[:, :, :], ALU.is_le, [B_sm, B_jb], [B_cmpj])
            P.op("vector", lambda e: e.reduce_sum(out=bef[:, :], in_=cmpj[:, :, :], axis=mybir.AxisListType.X), [B_cmpj], [B_bef])
            TS("vector", be_f[:, :], bef[:, :], float(NE - 1), None, ALU.min, None, [B_bef], [B_be])
            TS("vector", be1k[:, :], be_f[:, :], float(D), None, ALU.mult, None, [B_be], [B_be])
            TS("vector", be16[:, :], be_f[:, :], 16.0, None, ALU.mult, None, [B_be], [B_be])
            scat_tok = None
            for n in range(NTILE):
                for j in range(4):
                    scat_tok = P.dma("gpsimd", lambda e, n=n, j=j: e.indirect_dma_start(
                        out=slot_d[:, :], out_offset=bass.IndirectOffsetOnAxis(ap=idx[:, n, j:j + 1], axis=0),
                        in_=tokid[:, n:n + 1], in_offset=None), sem_for("scat"), [B_idx, B_tokid, B_slot], [Buf()])
            B_slot.w = scat_tok
            B_slot.r = {}

        P.barrier()
        B_Y = Buf()
        with ExitStack() as mo:
            def sbm(name, shape, dt=F32):
                return mo.enter_context(nc.sbuf_tensor(name, list(shape), dt))
            w1s = [sbm("w1s%d" % i, [128, KC, 2 * D], BF16) for i in range(2)]; B_w1s = [[Buf() for _ in range(KC)] for _ in range(2)]
            w2s = [sbm("w2s%d" % i, [128, KC, D], BF16) for i in range(2)]; B_w2s = [[Buf() for _ in range(KC)] for _ in range(2)]
            b1g = [sbm("b1g%d" % i, [16, 128]) for i in range(2)]; B_b1g = [Buf(), Buf()]
            b1T = [sbm("b1T%d" % i, [128, 32]) for i in range(2)]; B_b1T = [Buf(), Buf()]
            id16 = sbm("id16", [16, 16]); B_id16 = Buf()
            DMA(id16[:], cid_d[0:16, 0:16], "id16", [], [B_id16])
            b1rows = b1_d[0].rearrange("e (c f) -> (e c) f", f=128)
            sidx = [sbm("sidx%d" % i, [128, 4], I32) for i in range(2)]; B_sidx = [Buf(), Buf()]
            xg = [sbm("xg%d" % i, [128, 4, D], BF16) for i in range(2)]; B_xg = [[Buf() for _ in range(4)] for _ in range(2)]
            xgT = sbm("xgT", [128, KC, BLK], BF16); B_xgT = [Buf() for _ in range(KC // 2)]
            actT = sbm("actT", [128, KC, BLK], BF16); B_actT = [Buf() for _ in range(KC)]
            mt = [sbm("mt%d" % i, [128, 512]) for i in range(4)]; B_mt = [Buf() for _ in range(4)]
            yt = [sbm("yt%d" % i, [128, D]) for i in range(2)]; B_yt = [Buf(), Buf()]
            mt_i = [0]

            def nmt():
                i = mt_i[0] % 4
                mt_i[0] += 1
                return mt[i], B_mt[i]

            ridx = [sbm("ridx%d" % i, [128, KC], I32) for i in range(2)]; B_ridx = [Buf(), Buf()]
            eidx = [sbm("eidx%d" % i, [128, 1], I32) for i in range(2)]
            w1rows = w1_d[0].rearrange("e r n -> (e r) n")
            w2rows = w2_d[0].rearrange("e r n -> (e r) n")

            def bload(j, s_):
                TS("vector", ridx[s_][:, :], pkc[:, :], be1k[:, j:j + 1], None, ALU.add, None, [B_be, B_pkc], [B_ridx[s_]])
                TS("vector", eidx[s_][:, :], pkc[:, 0:1], be16[:, j:j + 1], None, ALU.add, None, [B_be, B_pkc], [B_ridx[s_]])
                P.dma("gpsimd", lambda e: e.indirect_dma_start(out=b1g[s_][0:16, :], out_offset=None, in_=b1rows,
                      in_offset=bass.IndirectOffsetOnAxis(ap=eidx[s_][0:16, 0:1], axis=0)), sem_for("b1g%d" % s_), [B_ridx[s_]], [B_b1g[s_]])
                for k in range(KC):
                    tok = P.dma("gpsimd", lambda e, k=k: e.indirect_dma_start(out=w1s[s_][:, k, :], out_offset=None, in_=w1rows,
                                in_offset=bass.IndirectOffsetOnAxis(ap=ridx[s_][:, k:k + 1], axis=0)), sem_for("w1s%d" % s_), [B_ridx[s_]], [B_w1s[s_][k]])
                for k in range(KC):
                    B_w1s[s_][k].w = tok
                for k in range(KC):
                    tok = P.dma("gpsimd", lambda e, k=k: e.indirect_dma_start(out=w2s[s_][:, k, :], out_offset=None, in_=w2rows,
                                in_offset=bass.IndirectOffsetOnAxis(ap=ridx[s_][:, k:k + 1], axis=0)), sem_for("w2s%d" % s_), [B_ridx[s_]], [B_w2s[s_][k]])
                for k in range(KC):
                    B_w2s[s_][k].w = tok
                DMA(sidx[s_][:], slot_d[j * BLK:(j + 1) * BLK, :].rearrange("(p q) o -> p (q o)", q=4), "sidx%d" % s_, [B_slot], [B_sidx[s_]])
                for q in range(4):
                    tok = P.dma("gpsimd", lambda e, q=q: e.indirect_dma_start(
                        out=xg[s_][:, q, :], out_offset=None, in_=h2_d[:, :],
                        in_offset=bass.IndirectOffsetOnAxis(ap=sidx[s_][:, q:q + 1], axis=0)),
                        sem_for("xg%d" % s_), [B_sidx[s_], B_h2row] + (B_h2tok if j == 0 else []), [B_xg[s_][q]])
                for q in range(4):
                    B_xg[s_][q].w = tok

            ytok = None
            ytoks = [None, None]
            yi = 0
            bload(0, 0)
            for j in range(NBLK):
                s_ = j % 2
                if j + 1 < NBLK:
                    bload(j + 1, (j + 1) % 2)
                psb_, bpb_ = nps()
                TR(psb_[:, 0:16], b1g[s_][0:16, :], id16[:, :], [B_b1g[s_], B_id16], [bpb_])
                CP("vector", b1T[s_][:, 0:16], psb_[:, 0:16], [bpb_], [B_b1T[s_]])
                TS("vector", b1T[s_][:, 16:32], psb_[:, 0:16], 1.0, None, ALU.add, None, [bpb_], [B_b1T[s_]])
                for kp in range(KC // 2):
                    ps, bp = nps()
                    psb = ps[:].bitcast(BF16)
                    for kk in range(2):
                        k = kp * 2 + kk
                        for q in range(4):
                            TR(psb[:, kk * 512 + q * 128:kk * 512 + (q + 1) * 128], xg[s_][:, q, k * 128:(k + 1) * 128], identb[:, :],
                               [B_xg[s_][q], B_identb], [bp])
                    CP("scalar" if kp % 2 else "vector", xgT[:, kp * 2:kp * 2 + 2, :].rearrange("p k s -> p (k s)"), psb[:, :], [bp], [B_xgT[kp]])
                for c in range(KC):
                    psg, bpg = nps()
                    for k in range(KC):
                        MM(psg[:, :], w1s[s_][:, k, c * 128:(c + 1) * 128], xgT[:, k, :], k == 0, k == KC - 1, [B_w1s[s_][k], B_xgT[k // 2]], [bpg])
                    psl, bpl = nps()
                    for k in range(KC):
                        MM(psl[:, :], w1s[s_][:, k, D + c * 128:D + (c + 1) * 128], xgT[:, k, :], k == 0, k == KC - 1, [B_w1s[s_][k], B_xgT[k // 2]], [bpl])
                    g_, bg = nmt(); sg, bsg = nmt(); l_, bl = nmt()
                    TS("vector", g_[:, :], psg[:, :], b1T[s_][:, c:c + 1], LIMIT, ALU.add, ALU.min, [bpg, B_b1T[s_]], [bg])
                    ACT(sg[:, :], g_[:, :], AF.Gelu_apprx_sigmoid, [bg], [bsg])
                    TS("vector", l_[:, :], psl[:, :], b1T[s_][:, 24 + c:25 + c], 1.0 - LIMIT, ALU.add, ALU.max, [bpl, B_b1T[s_]], [bl])
                    STT(actT[:, c, :], l_[:, :], 1.0 + LIMIT, sg[:, :], ALU.min, ALU.mult, [bl, bsg], [B_actT[c]])
                for q in range(4):
                    y_ = yt[yi % 2]; by = B_yt[yi % 2]
                    yi += 1
                    for n in range(2):
                        ps, bp = nps()
                        for k in range(KC):
                            MM(ps[:, :], actT[:, k, q * 128:(q + 1) * 128], w2s[s_][:, k, n * 512:(n + 1) * 512], k == 0, k == KC - 1, [B_actT[k], B_w2s[s_][k]], [bp])
                        CP("scalar", y_[:, n * 512:(n + 1) * 512], ps[:, :], [bp], [by])
                    ytok = DMA(Y_d[j * BLK:(j + 1) * BLK, :].rearrange("(p q) d -> p q d", q=4)[:, q, :], y_[:, :], "ys%d" % ((yi - 1) % 2), [by], [Buf()])
                    ytoks[(yi - 1) % 2] = ytok
            B_Y.w = ytoks[0]
            B_Y2 = Buf(); B_Y2.w = ytoks[1]

        P.barrier()
        with ExitStack() as cb:
            def sbc(name, shape, dt=F32):
                return cb.enter_context(nc.sbuf_tensor(name, list(shape), dt))
            yg = [sbc("yg%d" % i, [128, 4, D]) for i in range(2)]; B_yg = [[Buf() for _ in range(4)] for _ in range(2)]
            x1r = [sbc("x1r%d" % i, [128, D]) for i in range(2)]; B_x1r = [Buf(), Buf()]
            acc = [sbc("acc%d" % i, [128, D]) for i in range(2)]; B_acc = [Buf(), Buf()]
            oh = sbc("oh", [128, NE]); B_oh = Buf()
            g2c = sbc("g2c", [128, D]); B_g2c = Buf()
            b2sb = sbc("b2sb", [NE, D]); B_b2sb = Buf()
            idf = sbc("idf", [128, 128]); B_idf = Buf()
            wdT = sbc("wdT", [NE, 128]); B_wdT = Buf()
            DMA(b2sb[:], b2_d[0], "b2sb", [], [B_b2sb])
            DMA(idf[:], cid_d[:, :], "idf", [], [B_idf])
            wj = sbc("wj", [128, 8]); B_wj = Buf()
            jk = sbc("jk", [128, D], BF16); B_jk = Buf()
            fst = sbc("fst", [128, 8]); B_fst = Buf()
            B_out = [Buf() for _ in range(NTILE)]
            for tg in range(NTILE):
                s_ = tg % 2
                b = (tg * 128) // T
                if (tg * 128) % T == 0:
                    for n in range(2):
                        ps, bp = nps()
                        MM(ps[:, :], sel[0:R, b * 128:(b + 1) * 128], grow[:, 1, n * 512:(n + 1) * 512], True, True, [B_sel, B_grow], [bp])
                        CP("scalar", g2c[:, n * 512:(n + 1) * 512], ps[:, :], [bp], [B_g2c])
                for j in range(4):
                    tok = P.dma("gpsimd", lambda e, j=j, tg=tg, s_=s_: e.indirect_dma_start(
                        out=yg[s_][:, j, :], out_offset=None, in_=Y_d[:, :],
                        in_offset=bass.IndirectOffsetOnAxis(ap=idx[:, tg, j:j + 1], axis=0)),
                        sem_for("yg%d" % s_), [B_idx, B_Y, B_Y2], [B_yg[s_][j]])
                for j in range(4):
                    B_yg[s_][j].w = tok
                DMA(x1r[s_][:], x1_d[tg * 128:(tg + 1) * 128, :], "x1r%d" % s_, [B_x1d[tg]], [B_x1r[s_]])
                a_ = acc[s_]; ba = B_acc[s_]
                for j in range(4):
                    TS("vector", oh[:, :], vals[:, tg, :], dk[:, tg, j:j + 1], None, ALU.is_equal, None, [B_vals, B_dk], [B_oh])
                    TT("vector", oh[:, :], oh[:, :], wdense[:, tg, :], ALU.mult, [B_oh, B_wd[tg]], [B_oh])
                    P.op("vector", lambda e, j=j: e.reduce_sum(out=wj[:, j:j + 1], in_=oh[:, :], axis=mybir.AxisListType.X), [B_oh], [B_wj])
                ACT(a_[:, :], yg[s_][:, 0, :], AF.Copy, [B_yg[s_][0], B_wj], [ba], scale=wj[:, 0:1])
                for j in range(1, 4):
                    STT(a_[:, :], yg[s_][:, j, :], wj[:, j:j + 1], a_[:, :], ALU.mult, ALU.add, [B_yg[s_][j], B_wj, ba], [ba])
                pst, bpt = nps()
                TR(pst[0:NE, 0:128], wdense[:, tg, :], idf[:, :], [B_wd[tg], B_idf], [bpt])
                CP("scalar", wdT[:, :], pst[0:NE, 0:128], [bpt], [B_wdT])
                for n in range(2):
                    psn, bpn = nps()
                    MM(psn[:, :], wdT[:, :], b2sb[:, n * 512:(n + 1) * 512], True, True, [B_wdT, B_b2sb], [bpn])
                    TT("vector", a_[:, n * 512:(n + 1) * 512], a_[:, n * 512:(n + 1) * 512], psn[:, :], ALU.add, [ba, bpn], [ba])
                TT("gpsimd", a_[:, :], a_[:, :], g2c[:, :], ALU.mult, [ba, B_g2c], [ba])
                TT("vector", a_[:, :], a_[:, :], x1r[s_][:, :], ALU.add, [ba, B_x1r[s_]], [ba])
                ACT(jk[:, :], a_[:, :], AF.Square, [ba], [B_jk, B_fst], accum=fst[:, 0:1])
                ACT(fst[:, 1:2], fst[:, 0:1], AF.Ln, [B_fst], [B_fst], scale=1.0 / D, bias=epsc[:, 0:1])
                ACT(fst[:, 2:3], fst[:, 1:2], AF.Exp, [B_fst], [B_fst], scale=-0.5)
                STT(a_[:, :], a_[:, :], fst[:, 2:3], fgbc[:, :], ALU.mult, ALU.mult, [ba, B_fst, B_fgbc], [ba])
                DMA(out_d[tg * 128:(tg + 1) * 128, :], a_[:, :], "outs%d" % s_, [ba], [B_out[tg]])
            P.wait_all("sync", B_out)
        P.emit(es)
    return nc


def _consts(NTOK):
    ident = np.eye(128, dtype=np.float32)
    s = np.arange(128)[:, None]
    t = np.arange(128)[None, :]
    mf = (s <= t).astype(np.float32)
    mb = (s >= t).astype(np.float32)
    mask = np.concatenate([mf, mb], axis=1)
    sel = np.zeros((8, 8 * 128), np.float32)
    for r in range(8):
        sel[r, r * 128:(r + 1) * 128] = 1.0
    lst = (s < t).astype(np.float32)
    BLK = 512
    NBLK = (4 * NTOK) // BLK + NE
    JMAX = max(1, NTOK // BLK)
    jv = np.tile((BLK * np.arange(JMAX, dtype=np.float32))[None, None, :], (128, NE, 1)).reshape(128, NE * JMAX)
    jb = np.tile((BLK * np.arange(NBLK, dtype=np.float32))[None, :, None], (128, 1, NE)).reshape(128, NBLK * NE)
    ntile = NTOK // 128
    tokid = (np.arange(ntile, dtype=np.int32)[None, :] * 128 + np.arange(128, dtype=np.int32)[:, None]).astype(np.int32)
    pk = (np.arange(128, dtype=np.float32)[:, None] + 128.0 * np.arange(KC, dtype=np.float32)[None, :]).astype(np.float32)
    return dict(c_pk=np.ascontiguousarray(pk), c_ident=ident, c_mask=np.ascontiguousarray(mask), c_sel=sel, c_lst=np.ascontiguousarray(lst),
                c_jv=np.ascontiguousarray(jv), c_jb=np.ascontiguousarray(jb), c_tokid=np.ascontiguousarray(tokid))


def make_in_maps(inputs, n_cores, NB):
    T = inputs["x"].shape[1]
    consts = _consts(NB * T)
    maps = []
    for c in range(n_cores):
        bs = slice(c * NB, (c + 1) * NB)
        m = {k: np.ascontiguousarray(v) for k, v in inputs.items() if k not in ("x", "c", "ctx", "c_ctx")}
        m["x"] = np.ascontiguousarray(inputs["x"][bs])
        m["ctx"] = np.ascontiguousarray(inputs["ctx"][bs])
        m["cc"] = np.ascontiguousarray(np.concatenate([inputs["c"][bs], inputs["c_ctx"][None, :]], axis=0))
        m.update(consts)
        maps.append(m)
    return maps


def kernel(**inputs):
    inputs = {k: np.asarray(v, dtype=np.float32) for k, v in inputs.items()}
    n_cores = 8
    B, T, _ = inputs["x"].shape
    NB = B // n_cores
    nc = build_program(NB, T, inputs["ctx"].shape[1])
    maps = make_in_maps(inputs, n_cores, NB)
    res = run_bass_kernel_spmd(nc, maps, core_ids=list(range(n_cores)))
    out = np.concatenate([r["out"].reshape(NB, T, D) for r in res.results], axis=0)
    return out.astype(np.float32)
```

```python
from contextlib import ExitStack
import numpy as np
import concourse.bass as bass
import concourse.mybir as mybir
from concourse.bass_utils import run_bass_kernel_spmd

F32 = mybir.dt.float32
BF16 = mybir.dt.bfloat16
I32 = mybir.dt.int32
AF = mybir.ActivationFunctionType
ALU = mybir.AluOpType

ENGS = ("tensor", "vector", "scalar", "gpsimd", "sync")
D = 1024
KC = 8
H = 8
NE = 32
EPS = 1e-6
LIMIT = 7.0
ALPHA = 1.702


class Buf:
    __slots__ = ("name", "w", "r")

    def __init__(self, name=""):
        self.name = name
        self.w = None
        self.r = {}


class Prog:
    def __init__(self, nc):
        self.nc = nc
        self.q = {e: [] for e in ENGS}
        self.clock = {e: 0 for e in ENGS}
        self.seen = {e: {} for e in ENGS}
        self.dma_count = {}
        self.n_dma_sems = 0

    def new_dma_sem(self):
        k = "dma%d" % self.n_dma_sems
        self.n_dma_sems += 1
        self.dma_count[k] = 0
        return k

    def _need(self, eng, deps):
        for (k, v) in deps:
            if k == eng and eng == "tensor":
                continue
            if self.seen[eng].get(k, 0) >= v:
                continue
            if k in self.dma_count:
                v = self.dma_count[k]
            if self.seen[eng].get(k, 0) < v:
                self.seen[eng][k] = v
                self.q[eng].append(("wait", k, v))

    def _deps(self, reads, writes):
        deps = []
        for b in reads:
            if b.w is not None:
                deps.append(b.w)
        for b in writes:
            if b.w is not None:
                deps.append(b.w)
            deps.extend(b.r.items())
        return deps

    def op(self, eng, fn, reads=(), writes=()):
        self._need(eng, self._deps(reads, writes))
        self.clock[eng] += 1
        tok = (eng, self.clock[eng])
        self.q[eng].append(("op", fn, eng))
        for b in reads:
            if b.r.get(eng, 0) < tok[1]:
                b.r[eng] = tok[1]
        for b in writes:
            b.w = tok
            b.r = {}
        return tok

    def dma(self, eng, fn, sem, reads=(), writes=()):
        self._need(eng, self._deps(reads, writes))
        self.dma_count[sem] += 16
        tok = (sem, self.dma_count[sem])
        self.q[eng].append(("dma", fn, sem))
        for b in reads:
            if b.r.get(sem, 0) < tok[1]:
                b.r[sem] = tok[1]
        for b in writes:
            b.w = tok
            b.r = {}
        return tok

    def barrier(self):
        toks = [(e, self.clock[e]) for e in ENGS if self.clock[e] > 0] + [(k, v) for k, v in self.dma_count.items() if v > 0]
        for e in ENGS:
            self._need(e, [t for t in toks if t[0] != e])

    def wait_all(self, eng, bufs):
        self._need(eng, [b.w for b in bufs if b.w is not None])

    def emit(self, stack):
        nc = self.nc
        sems = {}
        for e in ENGS:
            sems[e] = stack.enter_context(nc.semaphore("s_" + e))
        for k in self.dma_count:
            sems[k] = stack.enter_context(nc.semaphore("s_" + k))
        block = stack.enter_context(nc.Block())
        q = self.q

        def replay(ename):
            def body(eng):
                for item in q[ename]:
                    if item[0] == "wait":
                        eng.wait_ge(sems[item[1]], item[2])
                    elif item[0] == "op":
                        item[1](eng).then_inc(sems[item[2]], 1)
                    else:
                        item[1](eng).then_inc(sems[item[2]], 16)
            return body

        block.tensor(replay("tensor"))
        block.vector(replay("vector"))
        block.scalar(replay("scalar"))
        block.gpsimd(replay("gpsimd"))
        block.sync(replay("sync"))


def build_program(NB, T, CTXL, debug=False):
    nc = bass.Bass("TRN2", target_bir_lowering=False)
    R = NB + 1
    NTOK = NB * T
    NTILE = NTOK // 128
    NCH_B = T // 128
    ST = 512
    assert T % ST == 0 and CTXL % 128 == 0 and CTXL <= ST
    NST = T // ST
    TG = 512
    NTG = NTOK // TG

    def din(name, shape, dt=F32):
        return nc.dram_tensor(name, list(shape), dt, kind="ExternalInput").ap()

    x_d = din("x", [NB, T, D])
    ctx_d = din("ctx", [NB, CTXL, D])
    cc_d = din("cc", [R, D])
    n1g_d = din("norm1_g", [1, D]); n2g_d = din("norm2_g", [1, D])
    wmod_d = din("w_mod", [1, D, 6 * D]); bmod_d = din("b_mod", [1, 6 * D])
    win_d = din("w_in", [1, D, 9 * D])
    lbf_d = din("lb_fwd", [2, D]); lbb_d = din("lb_bwd", [2, D])
    gng_d = din("gnorm_g", [1, 128])
    ws_d = din("w_s", [1, 8, 128, 128]); bs_d = din("b_s", [1, 8, 128])
    wba_d = din("w_branch_a", [1, D, D]); wbb_d = din("w_branch_b", [1, D, D]); wout_d = din("w_out", [1, D, D])
    wr_d = din("w_router", [1, D, NE]); br_d = din("b_router", [1, NE])
    w1_d = din("w1", [1, NE, D, 2 * D]); b1_d = din("b1", [1, NE, 2 * D])
    w2_d = din("w2", [1, NE, D, D]); b2_d = din("b2", [1, NE, D])
    fg_d = din("final_g", [D])
    cid_d = din("c_ident", [128, 128]); cmk_d = din("c_mask", [128, 256]); csel_d = din("c_sel", [8, 8 * 128])
    BLK_ = 512
    NBLK_ = (4 * NTOK) // BLK_ + NE - 1
    JMAX_ = max(1, NTOK // BLK_)
    clst_d = din("c_lst", [128, 128]); cjv_d = din("c_jv", [128, NE * JMAX_]); cjb_d = din("c_jb", [128, NBLK_ * NE])
    ctk_d = din("c_tokid", [128, NTILE], I32)
    cpk_d = din("c_pk", [128, KC])
    out_d = nc.dram_tensor("out", [NTOK, D], F32, kind="ExternalOutput").ap()
    dk = "ExternalOutput" if debug else "Internal"
    x1_d = nc.dram_tensor("x1s", [NTOK, D], F32, kind=dk).ap()
    h2_d = nc.dram_tensor("h2s", [NTOK + 1, D], BF16, kind=dk).ap()
    slot_d = nc.dram_tensor("slots", [NBLK_ * BLK_, 1], I32, kind=dk).ap()
    Y_d = nc.dram_tensor("ys", [NBLK_ * BLK_, D], F32, kind="Internal").ap()
    wd_d = nc.dram_tensor("wds", [128, NTILE * NE], F32, kind=dk).ap()
    dbg = {}
    if debug:
        for nm, dt_ in (("d_o", F32), ("d_ya", BF16), ("d_yb", BF16), ("d_mg", BF16), ("d_qs", BF16), ("d_it", BF16)):
            dbg[nm] = nc.dram_tensor(nm, [128, H * 512], dt_, kind="ExternalOutput").ap()
    wbf_d = nc.dram_tensor("wbf", [12, 128, KC, 1024], BF16, kind="Internal").ap()
    NSTT = NB * (T // 512)
    hTs_d = nc.dram_tensor("hTs", [NSTT, 128, KC * 512], BF16, kind="Internal").ap()
    its_d = nc.dram_tensor("its", [NSTT, 128, 4 * D], BF16, kind="Internal").ap()
    kes_d = nc.dram_tensor("kes", [NSTT, 128, H * 512], BF16, kind="Internal").ap()
    ebs_d = nc.dram_tensor("ebs", [NSTT, 128, H * 512], BF16, kind="Internal").ap()
    cns_d = nc.dram_tensor("cnss", [NSTT, 128, H * 12], F32, kind="Internal").ap()
    sp_d = nc.dram_tensor("spb", [NB * NCH_B, 128, H * 128], BF16, kind="Internal").ap()

    P = Prog(nc)
    with ExitStack() as es:
        def sb(name, shape, dt=F32):
            return es.enter_context(nc.sbuf_tensor(name, list(shape), dt))

        tm = ExitStack()

        def sbt(name, shape, dt=F32):
            return tm.enter_context(nc.sbuf_tensor(name, list(shape), dt))
        sel = sb("sel", [R, NB * 128]); B_sel = Buf()
        ones_f = sb("ones_f", [128, 128]); ones_b = sb("ones_b", [128, 512], BF16); zeros_f = sb("zeros_f", [128, 128])
        B_const = Buf()
        epsc = sb("epsc", [128, 1]); onec = sb("onec", [128, 1])
        grow = sb("grow", [R, 4, D]); B_grow = Buf()
        fgbc = sb("fgbc", [128, D]); B_fgbc = Buf()
        wdense = sb("wdense", [128, NTILE, NE]); B_wd = [Buf() for _ in range(NTILE)]
        maskall = sb("maskall", [128, NTILE, NE], BF16); B_maskall = Buf()
        identb = sb("identb", [128, 128], BF16); B_identb = Buf()
        B_h2tok = [Buf() for _ in range(NTILE)]; B_h2row = Buf()
        ident = sbt("ident", [128, 128]); B_ident = Buf()
        gcur = sbt("gcur", [128, 4, D]); B_gcur = [Buf() for _ in range(4)]
        maskf = sbt("maskf", [128, 256]); B_mask = Buf()
        vecT = sbt("vecT", [128, 64]); B_vecT = Buf()
        bmodT = sbt("bmodT", [128, 48]); B_bmodT = Buf()
        modT = sbt("modT", [128, 48, R]); B_modT = Buf()
        G1T = sbt("G1T", [128, R, 8]); G2T = sbt("G2T", [128, R, 8]); B_GT = Buf()
        lbs = sbt("lbs", [128, 2, 8]); B_lbs = Buf()
        wsT = sbt("wsT", [128, 8, 128], BF16); B_wsT = Buf()
        bsrow = sbt("bsrow", [1, 8 * 128], BF16); B_bsrow = Buf()
        wr_sb = sbt("wr_sb", [128, KC, NE]); B_wr = Buf()
        brrow = sbt("brrow", [1, NE]); B_br = Buf()
        S = sbt("S", [128, 2, H, 128]); B_S = [[Buf() for _ in range(H)] for _ in range(2)]
        psum = [es.enter_context(nc.psum_tensor("ps%d" % i, [128, 512], F32)) for i in range(8)]
        B_ps = [Buf() for _ in range(8)]
        ps_i = [0]

        def nps():
            i = ps_i[0] % 8
            ps_i[0] += 1
            return psum[i], B_ps[i]

        dsem = {}

        def sem_for(key):
            if key not in dsem:
                dsem[key] = P.new_dma_sem()
            return dsem[key]

        def ACT(out, in_, func, reads, writes, bias=None, scale=None, accum=None, eng="scalar"):
            kw = {}
            if bias is not None:
                kw["bias"] = bias
            if scale is not None:
                kw["scale"] = scale
            if accum is not None:
                kw["accum_out"] = accum
            return P.op(eng, lambda e: e.activation(out=out, in_=in_, func=func, **kw), reads, writes)

        def TS(eng, out, in0, s1, s2, op0, op1, reads, writes):
            if op1 is None:
                return P.op(eng, lambda e: e.tensor_scalar(out=out, in0=in0, scalar1=s1, scalar2=None, op0=op0), reads, writes)
            return P.op(eng, lambda e: e.tensor_scalar(out=out, in0=in0, scalar1=s1, scalar2=s2, op0=op0, op1=op1), reads, writes)

        def TT(eng, out, in0, in1, op, reads, writes):
            return P.op(eng, lambda e: e.tensor_tensor(out=out, in0=in0, in1=in1, op=op), reads, writes)

        def STT(out, in0, scalar, in1, op0, op1, reads, writes, eng="vector"):
            return P.op(eng, lambda e: e.scalar_tensor_tensor(out=out, in0=in0, scalar=scalar, in1=in1, op0=op0, op1=op1), reads, writes)

        def MM(out, lhsT, rhs, start, stop, reads, writes):
            return P.op("tensor", lambda e: e.matmul(out, lhsT, rhs, start=start, stop=stop), reads, writes)

        def TR(out, in_, idn, reads, writes):
            return P.op("tensor", lambda e: e.transpose(out, in_, idn), reads, writes)

        def CP(eng, out, in_, reads, writes):
            if eng == "scalar":
                return P.op(eng, lambda e: e.activation(out=out, in_=in_, func=AF.Copy), reads, writes)
            return P.op(eng, lambda e: e.tensor_copy(out=out, in_=in_), reads, writes)

        def DMA(out, in_, key, reads, writes, eng="sync"):
            return P.dma(eng, lambda e: e.dma_start(out=out, in_=in_), sem_for(key), reads, writes)

        def MEMSET(eng, t, val, writes):
            return P.op(eng, lambda e: e.memset(t, val), (), writes)

        DMA(ident[:], cid_d[:, :], "c0", [], [B_ident])
        DMA(maskf[:], cmk_d[:, :], "c1", [], [B_mask])
        DMA(sel[:], csel_d[0:R, 0:NB * 128], "c2", [], [B_sel])
        MEMSET("vector", ones_f[:], 1.0, [B_const])
        MEMSET("vector", ones_b[:], 1.0, [B_const])
        MEMSET("vector", zeros_f[:], 0.0, [B_const])
        MEMSET("vector", epsc[:], EPS, [B_const])
        MEMSET("vector", onec[:], 1.0, [B_const])
        CP("vector", identb[:], ident[:], [B_ident], [B_identb])
        DMA(fgbc[:], fg_d.rearrange("(o d) -> o d", o=1).partition_broadcast(128), "c3", [], [B_fgbc])
        DMA(brrow[:], br_d[:, :], "c4", [], [B_br])
        DMA(wr_sb[:], wr_d[0].rearrange("(k p) e -> p k e", p=128), "c5", [], [B_wr])

        B_wbf = [Buf() for _ in range(12)]
        with ExitStack() as s0:
            def sb0(name, shape, dt=F32):
                return s0.enter_context(nc.sbuf_tensor(name, list(shape), dt))
            stg = sb0("stg", [64, 128]); B_stg = Buf()
            stg2 = sb0("stg2", [48, 128]); B_stg2 = Buf()
            cc = sb0("cc_sb", [R, D]); B_cc = Buf()
            scc = sb0("scc", [R, D]); B_scc = Buf()
            scT = sb0("scT", [128, KC, R]); B_scT = Buf()
            wm = sb0("wm", [128, KC, 512]); B_wm = Buf()
            bmrow = sb0("bmrow", [1, 6 * D]); B_bmrow = Buf()
            wsf = sb0("wsf", [128, 8, 128]); B_wsf = Buf()
            bsf = sb0("bsf", [1, 8 * 128]); B_bsf = Buf()
            cst = sb0("cst", [128, KC, 1024], BF16); B_cst = [Buf(), Buf()]
            cst2 = sb0("cst2", [128, KC, 1024], BF16)
            csts = [cst, cst2]

            MEMSET("vector", stg[:], 0.0, [B_stg])
            DMA(stg[0:8, :], n1g_d[0].rearrange("(h d) -> h d", d=128), "s0", [], [B_stg])
            DMA(stg[8:16, :], n2g_d[0].rearrange("(h d) -> h d", d=128), "s0", [], [B_stg])
            DMA(stg[16:32, :], lbf_d.rearrange("r (h d) -> (r h) d", d=128), "s0", [], [B_stg])
            DMA(stg[32:48, :], lbb_d.rearrange("r (h d) -> (r h) d", d=128), "s0", [], [B_stg])
            DMA(stg[48:49, :], gng_d[:, :], "s0", [], [B_stg])
            DMA(stg2[:], bmod_d[0].rearrange("(j d) -> j d", d=128), "s1", [], [B_stg2])
            DMA(cc[:], cc_d[:, :], "s2", [], [B_cc])
            DMA(bmrow[:], bmod_d[:, :], "s3", [], [B_bmrow])
            DMA(wsf[:], ws_d[0].rearrange("g t p -> t g p"), "s4", [], [B_wsf])
            DMA(bsf[:], bs_d[0].rearrange("(o g) t -> o (g t)", o=1), "s5", [], [B_bsf])
            CP("vector", bsrow[:], bsf[:], [B_bsf], [B_bsrow])
            ps, bp = nps()
            TR(ps[:, 0:64], stg[:, :], ident[0:64, 0:64], [B_stg, B_ident], [bp])
            CP("vector", vecT[:], ps[:, 0:64], [bp], [B_vecT])
            ps, bp = nps()
            TR(ps[:, 0:48], stg2[:, :], ident[0:48, 0:48], [B_stg2, B_ident], [bp])
            CP("vector", bmodT[:], ps[:, 0:48], [bp], [B_bmodT])
            for g in range(8):
                ps, bp = nps()
                TR(ps[:, 0:128], wsf[:, g, :], ident[:, :], [B_wsf, B_ident], [bp])
                CP("vector", wsT[:, g, :], ps[:, 0:128], [bp], [B_wsT])
            for d_ in range(2):
                o = 16 + 16 * d_
                TT("vector", lbs[:, d_, :], vecT[:, o:o + 8], vecT[:, o + 8:o + 16], ALU.subtract, [B_vecT], [B_lbs])
            ACT(lbs[:], lbs[:], AF.Sigmoid, [B_lbs], [B_lbs])
            ACT(scc[:], cc[:], AF.Sigmoid, [B_cc], [B_scc])
            TT("vector", scc[:], scc[:], cc[:], ALU.mult, [B_scc, B_cc], [B_scc])
            for k in range(KC):
                ps, bp = nps()
                TR(ps[:, 0:R], scc[:, k * 128:(k + 1) * 128], ident[0:R, 0:R], [B_scc, B_ident], [bp])
                CP("vector", scT[:, k, :], ps[:, 0:R], [bp], [B_scT])
            for n in range(12):
                kind = n // 2
                DMA(wm[:], wmod_d[0, :, n * 512:(n + 1) * 512].rearrange("(k p) c -> p k c", p=128), "s6", [], [B_wm])
                if kind in (2, 3, 4, 5):
                    ps, bp = nps()
                    for k in range(KC):
                        MM(ps[0:R, :], scT[:, k, :], wm[:, k, :], k == 0, False, [B_scT, B_wm], [bp])
                    MM(ps[0:R, :], ones_f[0:1, 0:R], bmrow[0:1, n * 512:(n + 1) * 512], False, True, [B_const, B_bmrow], [bp])
                    CP("vector", grow[:, {2: 0, 5: 1, 3: 2, 4: 3}[kind], (n % 2) * 512:(n % 2) * 512 + 512], ps[0:R, :], [bp], [B_grow])
                if kind in (0, 1, 3, 4):
                    for jj in range(4):
                        j = n * 4 + jj
                        ps, bp = nps()
                        for k in range(KC):
                            MM(ps[:, 0:R], wm[:, k, jj * 128:(jj + 1) * 128], scT[:, k, :], k == 0, k == KC - 1, [B_scT, B_wm], [bp])
                        ACT(modT[:, j, :], ps[:, 0:R], AF.Identity, [bp, B_bmodT], [B_modT], bias=bmodT[:, j:j + 1])
            for r in range(R):
                for (GT, kind, vo) in ((G1T, 1, 0), (G2T, 4, 8)):
                    STT(GT[:, r, :], modT[:, kind * 8:kind * 8 + 8, r], 1.0, vecT[:, vo:vo + 8], ALU.add, ALU.mult, [B_modT, B_vecT], [B_GT])
            n2gR = sb0("n2gR", [R, D]); B_n2gR = Buf()
            DMA(n2gR[:], n2g_d[0:1, :].partition_broadcast(R).rearrange("p o d -> p (o d)"), "s7", [], [B_n2gR])
            STT(grow[:, 3, :], grow[:, 3, :], 1.0, n2gR[:, :], ALU.add, ALU.mult, [B_grow, B_n2gR], [B_grow])
            srcs = [win_d[0, :, g * 1024:(g + 1) * 1024] for g in range(9)] + [wba_d[0], wbb_d[0], wout_d[0]]
            for g, src in enumerate(srcs):
                c_ = csts[g % 2]
                DMA(c_[:], src.rearrange("(k p) n -> p k n", p=128), "cst%d" % (g % 2), [], [B_cst[g % 2]], eng="gpsimd")
                DMA(wbf_d[g], c_[:], "cstw%d" % (g % 2), [B_cst[g % 2]], [B_wbf[g]])
        P.barrier()
        with tm:
            xt = sbt("xt", [128, 4, D]); B_xt = [Buf() for _ in range(4)]
            stat = sbt("stat", [128, 16]); B_stat = Buf()
            hT = sbt("hT", [128, KC, ST], BF16); B_hT = [Buf() for _ in range(KC)]
            wsl = [sbt("wsl0", [128, KC, 1024], BF16), sbt("wsl1", [128, KC, 1024], BF16)]
            B_wsl = [Buf(), Buf()]
            qs = sbt("qs", [128, H, ST], BF16); B_qs = [Buf() for _ in range(H)]
            itok = sbt("itok", [128, 4, D], BF16); B_itok = [Buf() for _ in range(4)]
            qe = sbt("qe", [128, H, ST], BF16); B_qe = [Buf() for _ in range(H)]
            keT = sbt("keT", [128, H, ST], BF16); B_keT = [Buf() for _ in range(H)]
            ketok1 = sbt("ketok", [128, D], BF16); B_ketok1 = Buf()
            cns = sbt("cns", [128, H, 3, 4]); B_cns = [Buf() for _ in range(H)]
            sog = sbt("sog", [128, H, ST], BF16); B_sog = [Buf() for _ in range(H)]
            xtb = xt[:].rearrange("p c d -> p (c d)").bitcast(BF16)
            gu = xtb[:, 0:H * ST].rearrange("p (h t) -> p h t", t=ST); B_gu = [B_xt[0]] * 4 + [B_xt[1]] * 4
            vn = xtb[:, H * ST:2 * H * ST].rearrange("p (c d) -> p c d", d=D); B_vn = [B_xt[2], B_xt[2], B_xt[3], B_xt[3]]
            oT = sbt("oT", [128, H, ST]); B_oT = [Buf() for _ in range(H)]
            yaT = keT; B_yaT = B_keT
            ybT = qe; B_ybT = B_qe
            Sp = sbt("Sp", [128, H, 128], BF16); B_Sp = [Buf() for _ in range(H)]
            gv = sbt("gv", [128, D]); B_gv = Buf()
            xr = gv; B_xr = B_gv; junk = gv; B_junk = B_gv
            bst = sbt("bst", [128, 16]); B_bst = Buf()
            h2tok = sbt("h2tok", [128, D], BF16); B_h2t = Buf()
            rt = sbt("rt", [128, 4, NE]); B_rt = Buf()
            top8 = sbt("top8", [128, 16]); B_top8 = Buf()
            tmps = [sbt("tmp%d" % i, [128, ST]) for i in range(6)]; B_tmps = [Buf() for _ in range(6)]
            sTs = [sbt("sT%d" % i, [128, 4, 128], BF16) for i in range(2)]; B_sTs = [Buf(), Buf()]
            B_spd = [Buf() for _ in range(NB * NCH_B)]
            B_x1d = [Buf() for _ in range(NTILE)]
            B_scr = [[Buf() for _ in range(5)] for _ in range(NSTT)]
            tmp_i = [0]; st_i = [0]; w_i = [0]

            tmp_pool = [[(tmps[i], B_tmps[i]) for i in range(6)]]
            base_pool = tmp_pool[0]

            def ntmp():
                pool = tmp_pool[0]
                i = tmp_i[0] % len(pool)
                tmp_i[0] += 1
                return pool[i]

            def nst():
                i = st_i[0] % 2
                st_i[0] += 1
                return sTs[i], B_sTs[i]

            def wload(g):
                s_ = w_i[0] % 2
                w_i[0] += 1
                DMA(wsl[s_][:], wbf_d[g], "wsl%d" % s_, [B_wbf[g]], [B_wsl[s_]])
                return wsl[s_], B_wsl[s_]

            def v3(ap_, t=128):
                return ap_.rearrange("p (c t) -> p c t", t=t)

            def norm_T(r, nch, GT, shkind, outs):
                NT = nch * 128
                for c in range(nch):
                    ACT(junk[:], xt[:, c, :], AF.Square, [B_xt[c]], [B_junk, B_stat], accum=stat[:, c:c + 1])
                ACT(stat[:, 4:4 + nch], stat[:, 0:nch], AF.Ln, [B_stat], [B_stat], scale=1.0 / D, bias=epsc[:, 0:1])
                ACT(stat[:, 8:8 + nch], stat[:, 4:4 + nch], AF.Exp, [B_stat], [B_stat], scale=-0.5)
                for c in range(nch):
                    TS("vector", xt[:, c, :], xt[:, c, :], stat[:, 8 + c:9 + c], None, ALU.mult, None, [B_xt[c], B_stat], [B_xt[c]])
                for k in range(KC):
                    ps, bp = nps()
                    for c in range(nch):
                        TR(ps[:, c * 128:(c + 1) * 128], xt[:, c, k * 128:(k + 1) * 128], ident[:, :], [B_xt[c], B_ident], [bp])
                    for (ot, ob) in outs:
                        ACT(ot[:, k, 0:NT], ps[:, 0:NT], AF.Identity, [bp, B_GT, B_modT], [ob[k]],
                            scale=GT[:, r, k:k + 1], bias=modT[:, shkind * 8 + k, r:r + 1])

            def proj_fm(wt, bw, h, NT, src=None, bsrc=None):
                src = hT if src is None else src
                bsrc = B_hT if bsrc is None else bsrc
                ps, bp = nps()
                for k in range(KC):
                    MM(ps[:, 0:NT], wt[:, k, h * 128:(h + 1) * 128], src[:, k, 0:NT], k == 0, k == KC - 1, [bw, bsrc[k]], [bp])
                return ps, bp

            def proj_tm(wt, bw, c, n, src=None, bsrc=None):
                src = hT if src is None else src
                bsrc = B_hT if bsrc is None else bsrc
                ps, bp = nps()
                for k in range(KC):
                    MM(ps[:, :], src[:, k, c * 128:(c + 1) * 128], wt[:, k, n * 512:(n + 1) * 512], k == 0, k == KC - 1, [bw, bsrc[k]], [bp])
                return ps, bp

            def decay_prep(dr, h, ps, bp, nch, need_q):
                NT = nch * 128
                e_, be = ntmp(); l1, b1 = ntmp(); l2, b2 = ntmp()
                ACT(e_[:, 0:NT], ps[:, 0:NT], AF.Exp, [bp], [be], scale=-1.0)
                ACT(l1[:, 0:NT], e_[:, 0:NT], AF.Ln, [be], [b1], bias=onec[:, 0:1])
                ACT(l2[:, 0:NT], e_[:, 0:NT], AF.Ln, [be, B_lbs], [b2], scale=lbs[:, dr, h:h + 1], bias=onec[:, 0:1])
                yield
                TT("vector", l2[:, 0:NT], l2[:, 0:NT], l1[:, 0:NT], ALU.subtract, [b2, b1], [b2])
                ACT(l1[:, 0:NT], l2[:, 0:NT], AF.Exp, [b2], [b1])
                TS("gpsimd", l1[:, 0:NT], l1[:, 0:NT], -1.0, 1.0, ALU.mult, ALU.add, [b1], [b1])
                yield
                for c in range(nch):
                    cs = slice(c * 128, (c + 1) * 128)
                    P.op("vector", lambda e, cs=cs: e.tensor_tensor_scan(out=e_[:, cs], data0=l2[:, cs], data1=zeros_f[:, 0:128],
                                                                          initial=0.0, op0=ALU.add, op1=ALU.add), [b2, B_const], [be])
                yield
                P63b = v3(e_[:, 0:NT])[:, :, 63:64].to_broadcast([128, nch, 128])
                P63v = e_[:, 63:NT:128]; P127v = e_[:, 127:NT:128]
                if dr == 0:
                    TT("vector", v3(l2[:, 0:NT]), v3(e_[:, 0:NT]), P63b, ALU.subtract, [be], [b2])
                else:
                    TT("vector", l2[:, 0:NT], l2[:, 0:NT], e_[:, 0:NT], ALU.subtract, [be, b2], [b2])
                    TT("vector", v3(l2[:, 0:NT]), v3(l2[:, 0:NT]), P63b, ALU.add, [be, b2], [b2])
                ia, ib = (0, 1) if dr == 0 else (1, 0)
                ACT(cns[:, h, ia, 0:nch], P63v, AF.Exp, [be], [B_cns[h]])
                TT("vector", cns[:, h, ib, 0:nch], P127v, P63v, ALU.subtract, [be], [B_cns[h]])
                ACT(cns[:, h, ib, 0:nch], cns[:, h, ib, 0:nch], AF.Exp, [B_cns[h]], [B_cns[h]])
                ACT(cns[:, h, 2, 0:nch], P127v, AF.Exp, [be], [B_cns[h]])
                yield
                E_, bE = e_, be
                if need_q == "q":
                    ACT(E_[:, 0:NT], l2[:, 0:NT], AF.Exp, [b2], [bE])
                    TT("vector", qe[:, h, 0:NT], qs[:, h, 0:NT], E_[:, 0:NT], ALU.mult, [B_qs[h], bE], [B_qe[h]])
                elif need_q == "E":
                    ACT(qe[:, h, 0:NT], l2[:, 0:NT], AF.Exp, [b2], [B_qe[h]])
                ACT(E_[:, 0:NT], l2[:, 0:NT], AF.Exp, [b2], [bE], scale=-1.0)
                TT("gpsimd", keT[:, h, 0:NT], l1[:, 0:NT], E_[:, 0:NT], ALU.mult, [b1, bE], [B_keT[h]])

            def gla(dr, mode, nch, b, chunk0):
                order = range(nch) if dr == 0 else range(nch - 1, -1, -1)
                upd = (dr == 0) or (mode != "main")
                outp = mode == "main"
                for c in order:
                    cs = slice(c * 128, (c + 1) * 128)
                    cg = b * NCH_B + chunk0 + c
                    if upd:
                        ps, bp = nps()
                        psb = ps[:].bitcast(BF16)
                        for h in range(H):
                            TR(psb[:, h * 128:(h + 1) * 128], keT[:, h, cs], identb[:, :], [B_keT[h], B_identb], [bp])
                        CP("scalar", ketok1[:, :], psb[:, :], [bp], [B_ketok1])
                    if outp and dr == 1:
                        DMA(Sp[:].rearrange("p h v -> p (h v)"), sp_d[cg], "spl", [B_spd[cg]], B_Sp)
                    elif outp or mode == "pre":
                        for h in range(H):
                            if h % 2:
                                ACT(Sp[:, h, :], S[:, dr, h, :], AF.Copy, [B_S[dr][h], B_cns[h]], [B_Sp[h]], scale=cns[:, h, 0, c:c + 1])
                            else:
                                TS("vector", Sp[:, h, :], S[:, dr, h, :], cns[:, h, 0, c:c + 1], None, ALU.mult, None,
                                   [B_S[dr][h], B_cns[h]], [B_Sp[h]])
                        if mode == "pre":
                            DMA(sp_d[cg], Sp[:].rearrange("p h v -> p (h v)"), "sps", B_Sp, [B_spd[cg]])
                    if outp:
                        for hg in range(2):
                            hs = range(hg * 4, hg * 4 + 4)
                            ps, bp = nps()
                            for hh, h in enumerate(hs):
                                MM(ps[:, hh * 128:(hh + 1) * 128], keT[:, h, cs], qe[:, h, cs], True, True, [B_keT[h], B_qe[h]], [bp])
                            st_, bst_ = nst()
                            TT("vector", st_[:, :, :], v3(ps[:, :]), maskf[:, dr * 128:(dr + 1) * 128].unsqueeze(1).to_broadcast([128, 4, 128]), ALU.mult, [bp, B_mask], [bst_])
                            ps2, bp2 = nps()
                            for hh, h in enumerate(hs):
                                MM(ps2[:, hh * 128:(hh + 1) * 128], itok[:, c, h * 128:(h + 1) * 128], st_[:, hh, :], True, False, [B_itok[c], bst_], [bp2])
                                MM(ps2[:, hh * 128:(hh + 1) * 128], Sp[:, h, :], qe[:, h, cs], False, True, [B_Sp[h], B_qe[h]], [bp2])
                            bo = [B_oT[h] for h in hs]
                            if dr == 0:
                                CP("scalar", oT[:, hg * 4:hg * 4 + 4, cs], v3(ps2[:, :]), [bp2], bo)
                            else:
                                TT("vector", oT[:, hg * 4:hg * 4 + 4, cs], oT[:, hg * 4:hg * 4 + 4, cs], v3(ps2[:, :]), ALU.add, [bp2] + bo, bo)
                    if upd:
                        for hg in range(2):
                            ps, bp = nps()
                            for hh in range(4):
                                h = hg * 4 + hh
                                MM(ps[:, hh * 128:(hh + 1) * 128], ketok1[:, h * 128:(h + 1) * 128], itok[:, c, h * 128:(h + 1) * 128], True, True,
                                   [B_ketok1, B_itok[c]], [bp])
                            tu, btu = ntmp()
                            for hh in range(4):
                                h = hg * 4 + hh
                                ACT(tu[:, hh * 128:(hh + 1) * 128], ps[:, hh * 128:(hh + 1) * 128], AF.Copy, [bp, B_cns[h]], [btu], scale=cns[:, h, 1, c:c + 1])
                            for hh in range(4):
                                h = hg * 4 + hh
                                STT(S[:, dr, h, :], S[:, dr, h, :], cns[:, h, 2, c:c + 1], tu[:, hh * 128:(hh + 1) * 128], ALU.mult, ALU.add,
                                    [btu, B_cns[h], B_S[dr][h]], [B_S[dr][h]])
                    yield

            def interleave(gens):
                gens = list(gens)
                while gens:
                    for g_ in list(gens):
                        try:
                            next(g_)
                        except StopIteration:
                            gens.remove(g_)

            def st_pass(mode, b, r, src, nch, chunk0):
                NT = nch * 128
                sidx_ = b * NST + chunk0 // 4
                if mode == "main":
                    DMA(hT[:].rearrange("p k t -> p (k t)"), hTs_d[sidx_], "hTl", [B_scr[sidx_][0]], B_hT)
                    DMA(itok[:].rearrange("p c d -> p (c d)"), its_d[sidx_], "itl", [B_scr[sidx_][1]], B_itok)
                else:
                    DMA(xt[:, 0:nch, :], src.rearrange("(c p) d -> p c d", p=128), "xt", [], B_xt[0:nch])
                    norm_T(r, nch, G1T, 0, [(hT, B_hT)])
                    if mode == "pre":
                        DMA(hTs_d[sidx_], hT[:].rearrange("p k t -> p (k t)"), "hTs", B_hT, [B_scr[sidx_][0]])
                if mode == "main":
                    glist = [0, 2, 4, 5, 6, 9, 7, 10, 8, 11]
                elif mode == "pre":
                    glist = [1, 3]
                else:
                    glist = [1, 2, 3]
                pend = [wload(glist[0])]
                gi = [0]

                def wpre():
                    if gi[0] < len(glist):
                        pend[0] = wload(glist[gi[0]])

                def wnext(prefetch=True):
                    cur = pend[0]
                    gi[0] += 1
                    if prefetch:
                        wpre()
                    return cur

                if mode == "main":
                    wt, bw = wnext()
                    for h in range(H):
                        ps, bp = proj_fm(wt, bw, h, NT)
                        ACT(qs[:, h, 0:NT], ps[:, 0:NT], AF.Silu, [bp], [B_qs[h]])
                if mode != "main":
                    wt, bw = wnext()
                    for c in range(nch):
                        for n in range(2):
                            ps, bp = proj_tm(wt, bw, c, n)
                            CP("scalar" if n else "vector", itok[:, c, n * 512:(n + 1) * 512], ps[:, :], [bp], [B_itok[c]])
                    if mode == "pre":
                        DMA(its_d[sidx_], itok[:].rearrange("p c d -> p (c d)"), "its", B_itok, [B_scr[sidx_][1]])
                def fm_act(wt_, bw_, dst, bdst, func, per=2):
                    for h_ in range(H):
                        ps_, bp_ = proj_fm(wt_, bw_, h_, NT)
                        ACT(dst[:, h_, 0:NT], ps_[:, 0:NT], func, [bp_], [bdst[h_]])
                        if h_ % per == per - 1:
                            yield

                def decay_all(dr_, wt_, bw_):
                    if mode == "main":
                        extra = [(xt[:, c_, hf * 512:(hf + 1) * 512], B_xt[c_]) for c_ in range(4) for hf in range(2)]
                    else:
                        extra = [(oT[:, h_, :], B_oT[h_]) for h_ in range(H)]
                    tmp_pool[0] = base_pool + extra
                    for h0 in range(0, H, 4):
                        gs = []
                        for h_ in range(h0, h0 + 4):
                            ps_, bp_ = proj_fm(wt_, bw_, h_, NT)
                            gs.append(decay_prep(dr_, h_, ps_, bp_, nch, "q" if mode == "main" else ("E" if mode == "pre" else None)))
                        interleave(gs)
                    tmp_pool[0] = base_pool

                if mode != "main":
                    for dr in ((0, 1) if mode != "pre" else (1,)):
                        wt, bw = wnext()
                        decay_all(dr, wt, bw)
                        if mode == "pre":
                            DMA(kes_d[sidx_], keT[:].rearrange("p h t -> p (h t)"), "kes", B_keT, [B_scr[sidx_][2]])
                            DMA(ebs_d[sidx_], qe[:].rearrange("p h t -> p (h t)"), "ebs", B_qe, [B_scr[sidx_][3]])
                            DMA(cns_d[sidx_], cns[:].rearrange("p h a c -> p (h a c)"), "cnss", B_cns, [B_scr[sidx_][4]])
                        interleave([gla(dr, mode, nch, b, chunk0)])
                    return
                wt, bw = wnext()
                decay_all(0, wt, bw)
                wt, bw = wnext()
                interleave([gla(0, mode, nch, b, chunk0), fm_act(wt, bw, sog, B_sog, AF.Silu)])
                DMA(keT[:].rearrange("p h t -> p (h t)"), kes_d[sidx_], "kel", [B_scr[sidx_][2]], B_keT)
                DMA(qe[:].rearrange("p h t -> p (h t)"), ebs_d[sidx_], "ebl", [B_scr[sidx_][3]], B_qe)
                DMA(cns[:].rearrange("p h a c -> p (h a c)"), cns_d[sidx_], "cnl", [B_scr[sidx_][4]], B_cns)
                for h in range(H):
                    TT("gpsimd" if h % 2 else "vector", qe[:, h, 0:NT], qe[:, h, 0:NT], qs[:, h, 0:NT], ALU.mult, [B_qe[h], B_qs[h]], [B_qe[h]])
                wU, bU = wnext(); wV, bV = wnext(False)

                def v_gen():
                    for c in range(nch):
                        for n in range(2):
                            ps, bp = proj_tm(wV, bV, c, n)
                            ACT(gv[:, n * 512:(n + 1) * 512], ps[:, :], AF.Gelu, [bp], [B_gv])
                            P.op("vector", lambda e, n=n: e.bn_stats(out=bst[:, n * 6:(n + 1) * 6], in_=gv[:, n * 512:(n + 1) * 512]), [B_gv], [B_bst])
                        P.op("vector", lambda e: e.bn_aggr(out=bst[:, 12:14], in_=bst[:, 0:12]), [B_bst], [B_bst])
                        ACT(bst[:, 14:15], bst[:, 13:14], AF.Ln, [B_bst], [B_bst], bias=epsc[:, 0:1])
                        ACT(bst[:, 15:16], bst[:, 14:15], AF.Exp, [B_bst], [B_bst], scale=-0.5)
                        TS("vector", vn[:, c, :], gv[:, :], bst[:, 12:13], bst[:, 15:16], ALU.subtract, ALU.mult, [B_gv, B_bst], [B_vn[c]])
                        yield

                interleave([gla(1, mode, nch, b, chunk0), fm_act(wU, bU, gu, B_gu, AF.Gelu), v_gen()])
                wpre()
                for h in range(H):
                    sq, bsq = ntmp()
                    TT("gpsimd", sq[:, 0:NT], oT[:, h, 0:NT], oT[:, h, 0:NT], ALU.mult, [B_oT[h]], [bsq])
                    ps, bp = nps()
                    MM(ps[:, 0:NT], ones_f[:, 0:128], sq[:, 0:NT], True, True, [B_const, bsq], [bp])
                    l_, bl = ntmp()
                    ACT(l_[:, 0:NT], ps[:, 0:NT], AF.Ln, [bp], [bl], scale=1.0 / 128, bias=epsc[:, 0:1])
                    ACT(l_[:, 0:NT], l_[:, 0:NT], AF.Exp, [bl], [bl], scale=-0.5)
                    TT("vector", l_[:, 0:NT], l_[:, 0:NT], oT[:, h, 0:NT], ALU.mult, [bl, B_oT[h]], [bl])
                    STT(yaT[:, h, 0:NT], l_[:, 0:NT], vecT[:, 48:49], sog[:, h, 0:NT], ALU.mult, ALU.mult, [bl, B_vecT, B_sog[h]], [B_yaT[h]])
                for c in range(nch):
                    cs = slice(c * 128, (c + 1) * 128)
                    for hg in range(2):
                        ps, bp = nps()
                        for hh in range(4):
                            g = hg * 4 + hh
                            MM(ps[:, hh * 128:(hh + 1) * 128], vn[:, c, g * 128:(g + 1) * 128], wsT[:, g, :], True, False, [B_vn[c], B_wsT], [bp])
                            MM(ps[:, hh * 128:(hh + 1) * 128], ones_b[0:1, 0:128], bsrow[0:1, g * 128:(g + 1) * 128], False, True, [B_const, B_bsrow], [bp])
                        bo = [B_ybT[g] for g in range(hg * 4, hg * 4 + 4)]
                        TT("vector", ybT[:, hg * 4:hg * 4 + 4, cs], gu[:, hg * 4:hg * 4 + 4, cs], v3(ps[:, :]), ALU.mult,
                           [bp] + [B_gu[g] for g in range(hg * 4, hg * 4 + 4)], bo)
                if debug and b == 0 and chunk0 == 0:
                    DMA(dbg["d_ya"][:, :], yaT[:].rearrange("p h t -> p (h t)"), "dbg", B_yaT, [Buf()])
                    DMA(dbg["d_yb"][:, :], ybT[:].rearrange("p h t -> p (h t)"), "dbg", B_ybT, [Buf()])
                wA, bA = wnext(); wG, bG = wnext(False)
                for j in range(H):
                    psA, bpA = proj_fm(wA, bA, j, NT, yaT, B_yaT)
                    psG, bpG = proj_fm(wG, bG, j, NT)
                    sg, bsg = ntmp()
                    ACT(sg[:, 0:NT], psG[:, 0:NT], AF.Sigmoid, [bpG], [bsg])
                    TT("vector", oT[:, j, 0:NT], sg[:, 0:NT], psA[:, 0:NT], ALU.mult, [bsg, bpA], [B_oT[j]])
                wpre()
                wB, bB = wnext(); wG, bG = wnext(False)
                for j in range(H):
                    psB, bpB = proj_fm(wB, bB, j, NT, ybT, B_ybT)
                    psG, bpG = proj_fm(wG, bG, j, NT)
                    sg, bsg = ntmp()
                    ACT(sg[:, 0:NT], psG[:, 0:NT], AF.Sigmoid, [bpG], [bsg])
                    TT("vector", sg[:, 0:NT], sg[:, 0:NT], psB[:, 0:NT], ALU.mult, [bsg, bpB], [bsg])
                    TT("gpsimd", qs[:, j, 0:NT], sg[:, 0:NT], oT[:, j, 0:NT], ALU.add, [bsg, B_oT[j]], [B_qs[j]])
                if debug and b == 0 and chunk0 == 0:
                    DMA(dbg["d_mg"][:, :], qs[:].rearrange("p h t -> p (h t)"), "dbg", B_qs, [Buf()])
                wpre()
                wO, bO = wnext()
                for c in range(nch):
                    tg = (b * T) // 128 + chunk0 + c
                    DMA(xr[:], src[c * 128:(c + 1) * 128, :], "xr", [], [B_xr])
                    for n in range(2):
                        ps, bp = proj_tm(wO, bO, c, n, qs, B_qs)
                        t_, bt = ntmp()
                        TT("vector", t_[:, :], ps[:, :], gcur[:, 0, n * 512:(n + 1) * 512], ALU.mult, [bp, B_gcur[0]], [bt])
                        TT("gpsimd", xt[:, c, n * 512:(n + 1) * 512], t_[:, :], xr[:, n * 512:(n + 1) * 512], ALU.add, [bt, B_xr], [B_xt[c]])
                    DMA(x1_d[tg * 128:(tg + 1) * 128, :], xt[:, c, :], "x1s", [B_xt[c]], [B_x1d[tg]])
                norm_T(b, nch, G2T, 3, [(oT, B_oT)])
                for c in range(nch):
                    tg = (b * T) // 128 + chunk0 + c
                    TT("vector", gv[:, :], xt[:, c, :], gcur[:, 3, :], ALU.mult, [B_xt[c], B_gcur[3]], [B_gv])
                    TT("gpsimd", h2tok[:, :], gv[:, :], gcur[:, 2, :], ALU.add, [B_gv, B_gcur[2]], [B_h2t])
                    DMA(h2_d[tg * 128:(tg + 1) * 128, :], h2tok[:, :], "h2s", [B_h2t], [B_h2tok[tg]])
                for c in range(nch):
                    cs = slice(c * 128, (c + 1) * 128)
                    tg = (b * T) // 128 + chunk0 + c
                    ps, bp = nps()
                    for k in range(KC):
                        MM(ps[:, 0:NE], oT[:, k, cs], wr_sb[:, k, :], k == 0, False, [B_oT[k], B_wr], [bp])
                    MM(ps[:, 0:NE], ones_f[0:1, 0:128], brrow[0:1, :], False, True, [B_const, B_br], [bp])
                    lg = rt[:, 0, :]; mk = rt[:, 1, :]; ex = rt[:, 2, :]
                    CP("vector", lg, ps[:, 0:NE], [bp], [B_rt])
                    P.op("vector", lambda e, lg=lg: e.max(out=top8[:, 0:8], in_=lg), [B_rt], [B_top8])
                    TS("vector", mk, lg, top8[:, 3:4], None, ALU.is_ge, None, [B_rt, B_top8], [B_rt])
                    CP("gpsimd", maskall[:, tg, :], mk, [B_rt], [B_maskall])
                    TS("vector", top8[:, 8:9], top8[:, 0:1], -1.0, None, ALU.mult, None, [B_top8], [B_top8])
                    ACT(ex, lg, AF.Exp, [B_rt, B_top8], [B_rt], bias=top8[:, 8:9])
                    TT("vector", ex, ex, mk, ALU.mult, [B_rt], [B_rt])
                    P.op("vector", lambda e, ex=ex: e.reduce_sum(out=top8[:, 9:10], in_=ex, axis=mybir.AxisListType.X), [B_rt], [B_top8])
                    P.op("vector", lambda e: e.reciprocal(out=top8[:, 10:11], in_=top8[:, 9:10]), [B_top8], [B_top8])
                    TS("vector", wdense[:, tg, :], ex, top8[:, 10:11], None, ALU.mult, None, [B_rt, B_top8], [B_wd[tg]])

            def bcast_g(b, which):
                for n in range(2):
                    ps, bp = nps()
                    MM(ps[:, :], sel[0:R, b * 128:(b + 1) * 128], grow[:, which, n * 512:(n + 1) * 512], True, True, [B_sel, B_grow], [bp])
                    CP("scalar", gcur[:, which, n * 512:(n + 1) * 512], ps[:, :], [bp], [B_gcur[which]])

            for b in range(NB):
                for dr in range(2):
                    for h in range(H):
                        MEMSET("gpsimd", S[:, dr, h, :], 0.0, [B_S[dr][h]])
                bcast_g(b, 0); bcast_g(b, 2); bcast_g(b, 3)
                st_pass("ctx", b, NB, ctx_d[b], CTXL // 128, 0)
                for sti in range(NST - 1, -1, -1):
                    st_pass("pre", b, b, x_d[b, sti * ST:(sti + 1) * ST, :], 4, sti * 4)
                for sti in range(NST):
                    st_pass("main", b, b, x_d[b, sti * ST:(sti + 1) * ST, :], 4, sti * 4)
            if debug:
                DMA(wd_d[:, :], wdense[:].rearrange("p t e -> p (t e)"), "wds", B_wd, [Buf()])

        P.barrier()
        BLK = 512
        NBLK = (4 * NTOK) // BLK + NE - 1
        CAP = NBLK * BLK
        JMAX = NTOK // BLK if NTOK >= BLK else 1
        vals = sb("vals", [128, NTILE, NE]); B_vals = Buf()
        dk = sb("dk", [128, NTILE, 8]); B_dk = Buf()
        idx = sb("idx", [128, NTILE, 4], I32); B_idx = Buf()
        be_f = sb("be_f", [128, NBLK]); be1k = sb("be1k", [128, NBLK]); be16 = sb("be16", [128, NBLK]); B_be = Buf()
        pkc = sb("pkc", [128, KC]); B_pkc = Buf()
        DMA(pkc[:], cpk_d[:, :], "c7", [], [B_pkc])
        tokid = sb("tokid", [128, NTILE], I32); B_tokid = Buf()
        DMA(tokid[:], ctk_d[:, :], "c6", [], [B_tokid])
        B_slot = Buf()
        with ExitStack() as ra:
            def sbr(name, shape, dt=F32):
                return ra.enter_context(nc.sbuf_tensor(name, list(shape), dt))
            lst = sbr("lst", [128, 128]); lstb = sbr("lstb", [128, 128], BF16); B_lst = Buf()
            cnt_all = sbr("cnt_all", [128, NTILE, NE]); B_cnt = Buf()
            base_all = sbr("base_all", [128, NTILE, NE]); B_base = Buf()
            jv = sbr("jv", [128, NE, JMAX]); B_jv = Buf()
            jb = sbr("jb", [128, NBLK, NE]); B_jb = Buf()
            cmpj = sbr("cmpj", [128, NBLK, NE]); B_cmpj = Buf()
            sm = sbr("sm", [128, 8, NE]); B_sm = Buf()
            sent = sbr("sent", [128, CAP // 128], I32); sentf = sbr("sentf", [128, CAP // 128]); B_sent = Buf()
            bef = sbr("bef", [128, NBLK]); B_bef = Buf()
            DMA(lst[:], clst_d[:, :], "r0", [], [B_lst])
            CP("vector", lstb[:], lst[:], [B_lst], [B_lst])
            DMA(jv[:], cjv_d[:, :].rearrange("p (e j) -> p e j", j=JMAX), "r1", [], [B_jv])
            DMA(jb[:], cjb_d[:, :].rearrange("p (j e) -> p j e", e=NE), "r2", [], [B_jb])
            MEMSET("vector", sentf[:], float(NTOK), [B_sent])
            CP("vector", sent[:], sentf[:], [B_sent], [B_sent])
            DMA(slot_d.rearrange("(p q) o -> p (q o)", p=128), sent[:], "r3", [B_sent], [B_slot])
            zrow = sbr("zrow", [1, 512]); B_zrow = Buf()
            MEMSET("vector", zrow[:], 0.0, [B_zrow])
            DMA(h2_d[NTOK:NTOK + 1, :], zrow[0:1, :].bitcast(BF16), "r4", [B_zrow], [B_h2row])
            GT_ = 16
            for g0 in range(0, NTILE, GT_):
                gn = min(GT_, NTILE - g0)
                rhs = maskall[:, g0:g0 + gn, :].rearrange("p n e -> p (n e)")
                ps, bp = nps()
                MM(ps[:, 0:gn * NE], lstb[:, :], rhs, True, True, [B_lst, B_maskall], [bp])
                CP("vector", vals[:, g0:g0 + gn, :].rearrange("p n e -> p (n e)"), ps[:, 0:gn * NE], [bp], [B_vals])
                ps2, bp2 = nps()
                MM(ps2[:, 0:gn * NE], ones_b[:, 0:128], rhs, True, True, [B_const, B_maskall], [bp2])
                CP("scalar", cnt_all[:, g0:g0 + gn, :].rearrange("p n e -> p (n e)"), ps2[:, 0:gn * NE], [bp2], [B_cnt])
            MEMSET("vector", base_all[:, 0, :], 0.0, [B_base])
            for n in range(1, NTILE):
                TT("vector", base_all[:, n, :], base_all[:, n - 1, :], cnt_all[:, n - 1, :], ALU.add, [B_base, B_cnt], [B_base])
            tot = sm[:, 0, :]; nbk = sm[:, 1, :]; pend_ = sm[:, 2, :]; pst = sm[:, 3, :]
            TT("vector", tot, base_all[:, NTILE - 1, :], cnt_all[:, NTILE - 1, :], ALU.add, [B_base, B_cnt], [B_sm])
            TT("vector", cmpj[:, 0:NE, 0:JMAX], tot.unsqueeze(2).to_broadcast([128, NE, JMAX]), jv[:, :, :], ALU.is_gt, [B_sm, B_jv], [B_cmpj])
            P.op("vector", lambda e: e.reduce_sum(out=nbk, in_=cmpj[:, 0:NE, 0:JMAX], axis=mybir.AxisListType.X), [B_cmpj], [B_sm])
            TS("vector", nbk, nbk, float(BLK), None, ALU.mult, None, [B_sm], [B_sm])
            P.op("vector", lambda e: e.tensor_tensor_scan(out=pend_, data0=nbk, data1=zeros_f[:, 0:NE], initial=0.0, op0=ALU.add, op1=ALU.add),
                 [B_sm, B_const], [B_sm])
            TT("vector", pst, pend_, nbk, ALU.subtract, [B_sm], [B_sm])
            TT("vector", vals[:, :, :], vals[:, :, :], base_all[:, :, :], ALU.add, [B_vals, B_base], [B_vals])
            TT("vector", vals[:, :, :], vals[:, :, :], pst.unsqueeze(1).to_broadcast([128, NTILE, NE]), ALU.add, [B_vals, B_sm], [B_vals])
            STT(vals[:, :, :], vals[:, :, :], 1.0, maskall[:, :, :], ALU.add, ALU.mult, [B_vals, B_maskall], [B_vals])
            for n in range(NTILE):
                P.op("vector", lambda e, n=n: e.max(out=dk[:, n, :], in_=vals[:, n, :]), [B_vals], [B_dk])
            TS("vector", idx[:, :, :], dk[:, :, 0:4], -1.0, None, ALU.add, None, [B_dk], [B_idx])
            TT("vector", cmpj[:, :, :], pend_.unsqueeze(1).to_broadcast([128, NBLK, NE]), jb[:, :, :], ALU.is_le, [B_sm, B_jb], [B_cmpj])
            P.op("vector", lambda e: e.reduce_sum(out=bef[:, :], in_=cmpj[:, :, :], axis=mybir.AxisListType.X), [B_cmpj], [B_bef])
            TS("vector", be_f[:, :], bef[:, :], float(NE - 1), None, ALU.min, None, [B_bef], [B_be])
            TS("vector", be1k[:, :], be_f[:, :], float(D), None, ALU.mult, None, [B_be], [B_be])
            TS("vector", be16[:, :], be_f[:, :], 16.0, None, ALU.mult, None, [B_be], [B_be])
            scat_tok = None
            for n in range(NTILE):
                for j in range(4):
                    scat_tok = P.dma("gpsimd", lambda e, n=n, j=j: e.indirect_dma_start(
                        out=slot_d[:, :], out_offset=bass.IndirectOffsetOnAxis(ap=idx[:, n, j:j + 1], axis=0),
                        in_=tokid[:, n:n + 1], in_offset=None), sem_for("scat"), [B_idx, B_tokid, B_slot], [Buf()])
            B_slot.w = scat_tok
            B_slot.r = {}

        P.barrier()
        B_Y = Buf()
        with ExitStack() as mo:
            def sbm(name, shape, dt=F32):
                return mo.enter_context(nc.sbuf_tensor(name, list(shape), dt))
            w1s = [sbm("w1s%d" % i, [128, KC, 2 * D], BF16) for i in range(2)]; B_w1s = [[Buf() for _ in range(KC)] for _ in range(2)]
            w2s = [sbm("w2s%d" % i, [128, KC, D], BF16) for i in range(2)]; B_w2s = [[Buf() for _ in range(KC)] for _ in range(2)]
            b1g = [sbm("b1g%d" % i, [16, 128]) for i in range(2)]; B_b1g = [Buf(), Buf()]
            b1T = [sbm("b1T%d" % i, [128, 32]) for i in range(2)]; B_b1T = [Buf(), Buf()]
            id16 = sbm("id16", [16, 16]); B_id16 = Buf()
            DMA(id16[:], cid_d[0:16, 0:16], "id16", [], [B_id16])
            b1rows = b1_d[0].rearrange("e (c f) -> (e c) f", f=128)
            sidx = [sbm("sidx%d" % i, [128, 4], I32) for i in range(2)]; B_sidx = [Buf(), Buf()]
            xg = [sbm("xg%d" % i, [128, 4, D], BF16) for i in range(2)]; B_xg = [[Buf() for _ in range(4)] for _ in range(2)]
            xgT = sbm("xgT", [128, KC, BLK], BF16); B_xgT = [Buf() for _ in range(KC // 2)]
            actT = sbm("actT", [128, KC, BLK], BF16); B_actT = [Buf() for _ in range(KC)]
            mt = [sbm("mt%d" % i, [128, 512]) for i in range(4)]; B_mt = [Buf() for _ in range(4)]
            yt = [sbm("yt%d" % i, [128, D]) for i in range(2)]; B_yt = [Buf(), Buf()]
            mt_i = [0]

            def nmt():
                i = mt_i[0] % 4
                mt_i[0] += 1
                return mt[i], B_mt[i]

            ridx = [sbm("ridx%d" % i, [128, KC], I32) for i in range(2)]; B_ridx = [Buf(), Buf()]
            eidx = [sbm("eidx%d" % i, [128, 1], I32) for i in range(2)]
            w1rows = w1_d[0].rearrange("e r n -> (e r) n")
            w2rows = w2_d[0].rearrange("e r n -> (e r) n")

            def bload(j, s_):
                TS("vector", ridx[s_][:, :], pkc[:, :], be1k[:, j:j + 1], None, ALU.add, None, [B_be, B_pkc], [B_ridx[s_]])
                TS("vector", eidx[s_][:, :], pkc[:, 0:1], be16[:, j:j + 1], None, ALU.add, None, [B_be, B_pkc], [B_ridx[s_]])
                P.dma("gpsimd", lambda e: e.indirect_dma_start(out=b1g[s_][0:16, :], out_offset=None, in_=b1rows,
                      in_offset=bass.IndirectOffsetOnAxis(ap=eidx[s_][0:16, 0:1], axis=0)), sem_for("b1g%d" % s_), [B_ridx[s_]], [B_b1g[s_]])
                for k in range(KC):
                    tok = P.dma("gpsimd", lambda e, k=k: e.indirect_dma_start(out=w1s[s_][:, k, :], out_offset=None, in_=w1rows,
                                in_offset=bass.IndirectOffsetOnAxis(ap=ridx[s_][:, k:k + 1], axis=0)), sem_for("w1s%d" % s_), [B_ridx[s_]], [B_w1s[s_][k]])
                for k in range(KC):
                    B_w1s[s_][k].w = tok
                for k in range(KC):
                    tok = P.dma("gpsimd", lambda e, k=k: e.indirect_dma_start(out=w2s[s_][:, k, :], out_offset=None, in_=w2rows,
                                in_offset=bass.IndirectOffsetOnAxis(ap=ridx[s_][:, k:k + 1], axis=0)), sem_for("w2s%d" % s_), [B_ridx[s_]], [B_w2s[s_][k]])
                for k in range(KC):
                    B_w2s[s_][k].w = tok
                DMA(sidx[s_][:], slot_d[j * BLK:(j + 1) * BLK, :].rearrange("(p q) o -> p (q o)", q=4), "sidx%d" % s_, [B_slot], [B_sidx[s_]])
                for q in range(4):
                    tok = P.dma("gpsimd", lambda e, q=q: e.indirect_dma_start(
                        out=xg[s_][:, q, :], out_offset=None, in_=h2_d[:, :],
                        in_offset=bass.IndirectOffsetOnAxis(ap=sidx[s_][:, q:q + 1], axis=0)),
                        sem_for("xg%d" % s_), [B_sidx[s_], B_h2row] + (B_h2tok if j == 0 else []), [B_xg[s_][q]])
                for q in range(4):
                    B_xg[s_][q].w = tok

            ytok = None
            ytoks = [None, None]
            yi = 0
            bload(0, 0)
            for j in range(NBLK):
                s_ = j % 2
                if j + 1 < NBLK:
                    bload(j + 1, (j + 1) % 2)
                psb_, bpb_ = nps()
                TR(psb_[:, 0:16], b1g[s_][0:16, :], id16[:, :], [B_b1g[s_], B_id16], [bpb_])
                CP("vector", b1T[s_][:, 0:16], psb_[:, 0:16], [bpb_], [B_b1T[s_]])
                TS("vector", b1T[s_][:, 16:32], psb_[:, 0:16], 1.0, None, ALU.add, None, [bpb_], [B_b1T[s_]])
                for kp in range(KC // 2):
                    ps, bp = nps()
                    psb = ps[:].bitcast(BF16)
                    for kk in range(2):
                        k = kp * 2 + kk
                        for q in range(4):
                            TR(psb[:, kk * 512 + q * 128:kk * 512 + (q + 1) * 128], xg[s_][:, q, k * 128:(k + 1) * 128], identb[:, :],
                               [B_xg[s_][q], B_identb], [bp])
                    CP("scalar" if kp % 2 else "vector", xgT[:, kp * 2:kp * 2 + 2, :].rearrange("p k s -> p (k s)"), psb[:, :], [bp], [B_xgT[kp]])
                for c in range(KC):
                    psg, bpg = nps()
                    for k in range(KC):
                        MM(psg[:, :], w1s[s_][:, k, c * 128:(c + 1) * 128], xgT[:, k, :], k == 0, k == KC - 1, [B_w1s[s_][k], B_xgT[k // 2]], [bpg])
                    psl, bpl = nps()
                    for k in range(KC):
                        MM(psl[:, :], w1s[s_][:, k, D + c * 128:D + (c + 1) * 128], xgT[:, k, :], k == 0, k == KC - 1, [B_w1s[s_][k], B_xgT[k // 2]], [bpl])
                    g_, bg = nmt(); sg, bsg = nmt(); l_, bl = nmt()
                    TS("vector", g_[:, :], psg[:, :], b1T[s_][:, c:c + 1], LIMIT, ALU.add, ALU.min, [bpg, B_b1T[s_]], [bg])
                    ACT(sg[:, :], g_[:, :], AF.Gelu_apprx_sigmoid, [bg], [bsg])
                    TS("vector", l_[:, :], psl[:, :], b1T[s_][:, 24 + c:25 + c], 1.0 - LIMIT, ALU.add, ALU.max, [bpl, B_b1T[s_]], [bl])
                    STT(actT[:, c, :], l_[:, :], 1.0 + LIMIT, sg[:, :], ALU.min, ALU.mult, [bl, bsg], [B_actT[c]])
                for q in range(4):
                    y_ = yt[yi % 2]; by = B_yt[yi % 2]
                    yi += 1
                    for n in range(2):
                        ps, bp = nps()
                        for k in range(KC):
                            MM(ps[:, :], actT[:, k, q * 128:(q + 1) * 128], w2s[s_][:, k, n * 512:(n + 1) * 512], k == 0, k == KC - 1, [B_actT[k], B_w2s[s_][k]], [bp])
                        CP("scalar", y_[:, n * 512:(n + 1) * 512], ps[:, :], [bp], [by])
                    ytok = DMA(Y_d[j * BLK:(j + 1) * BLK, :].rearrange("(p q) d -> p q d", q=4)[:, q, :], y_[:, :], "ys%d" % ((yi - 1) % 2), [by], [Buf()])
                    ytoks[(yi - 1) % 2] = ytok
            B_Y.w = ytoks[0]
            B_Y2 = Buf(); B_Y2.w = ytoks[1]

        P.barrier()
        with ExitStack() as cb:
            def sbc(name, shape, dt=F32):
                return cb.enter_context(nc.sbuf_tensor(name, list(shape), dt))
            yg = [sbc("yg%d" % i, [128, 4, D]) for i in range(2)]; B_yg = [[Buf() for _ in range(4)] for _ in range(2)]
            x1r = [sbc("x1r%d" % i, [128, D]) for i in range(2)]; B_x1r = [Buf(), Buf()]
            acc = [sbc("acc%d" % i, [128, D]) for i in range(2)]; B_acc = [Buf(), Buf()]
            oh = sbc("oh", [128, NE]); B_oh = Buf()
            g2c = sbc("g2c", [128, D]); B_g2c = Buf()
            b2sb = sbc("b2sb", [NE, D]); B_b2sb = Buf()
            idf = sbc("idf", [128, 128]); B_idf = Buf()
            wdT = sbc("wdT", [NE, 128]); B_wdT = Buf()
            DMA(b2sb[:], b2_d[0], "b2sb", [], [B_b2sb])
            DMA(idf[:], cid_d[:, :], "idf", [], [B_idf])
            wj = sbc("wj", [128, 8]); B_wj = Buf()
            jk = sbc("jk", [128, D], BF16); B_jk = Buf()
            fst = sbc("fst", [128, 8]); B_fst = Buf()
            B_out = [Buf() for _ in range(NTILE)]
            for tg in range(NTILE):
                s_ = tg % 2
                b = (tg * 128) // T
                if (tg * 128) % T == 0:
                    for n in range(2):
                        ps, bp = nps()
                        MM(ps[:, :], sel[0:R, b * 128:(b + 1) * 128], grow[:, 1, n * 512:(n + 1) * 512], True, True, [B_sel, B_grow], [bp])
                        CP("scalar", g2c[:, n * 512:(n + 1) * 512], ps[:, :], [bp], [B_g2c])
                for j in range(4):
                    tok = P.dma("gpsimd", lambda e, j=j, tg=tg, s_=s_: e.indirect_dma_start(
                        out=yg[s_][:, j, :], out_offset=None, in_=Y_d[:, :],
                        in_offset=bass.IndirectOffsetOnAxis(ap=idx[:, tg, j:j + 1], axis=0)),
                        sem_for("yg%d" % s_), [B_idx, B_Y, B_Y2], [B_yg[s_][j]])
                for j in range(4):
                    B_yg[s_][j].w = tok
                DMA(x1r[s_][:], x1_d[tg * 128:(tg + 1) * 128, :], "x1r%d" % s_, [B_x1d[tg]], [B_x1r[s_]])
                a_ = acc[s_]; ba = B_acc[s_]
                for j in range(4):
                    TS("vector", oh[:, :], vals[:, tg, :], dk[:, tg, j:j + 1], None, ALU.is_equal, None, [B_vals, B_dk], [B_oh])
                    TT("vector", oh[:, :], oh[:, :], wdense[:, tg, :], ALU.mult, [B_oh, B_wd[tg]], [B_oh])
                    P.op("vector", lambda e, j=j: e.reduce_sum(out=wj[:, j:j + 1], in_=oh[:, :], axis=mybir.AxisListType.X), [B_oh], [B_wj])
                ACT(a_[:, :], yg[s_][:, 0, :], AF.Copy, [B_yg[s_][0], B_wj], [ba], scale=wj[:, 0:1])
                for j in range(1, 4):
                    STT(a_[:, :], yg[s_][:, j, :], wj[:, j:j + 1], a_[:, :], ALU.mult, ALU.add, [B_yg[s_][j], B_wj, ba], [ba])
                pst, bpt = nps()
                TR(pst[0:NE, 0:128], wdense[:, tg, :], idf[:, :], [B_wd[tg], B_idf], [bpt])
                CP("scalar", wdT[:, :], pst[0:NE, 0:128], [bpt], [B_wdT])
                for n in range(2):
                    psn, bpn = nps()
                    MM(psn[:, :], wdT[:, :], b2sb[:, n * 512:(n + 1) * 512], True, True, [B_wdT, B_b2sb], [bpn])
                    TT("vector", a_[:, n * 512:(n + 1) * 512], a_[:, n * 512:(n + 1) * 512], psn[:, :], ALU.add, [ba, bpn], [ba])
                TT("vector", a_[:, :], a_[:, :], g2c[:, :], ALU.mult, [ba, B_g2c], [ba])
                TT("vector", a_[:, :], a_[:, :], x1r[s_][:, :], ALU.add, [ba, B_x1r[s_]], [ba])
                ACT(jk[:, :], a_[:, :], AF.Square, [ba], [B_jk, B_fst], accum=fst[:, 0:1])
                ACT(fst[:, 1:2], fst[:, 0:1], AF.Ln, [B_fst], [B_fst], scale=1.0 / D, bias=epsc[:, 0:1])
                ACT(fst[:, 2:3], fst[:, 1:2], AF.Exp, [B_fst], [B_fst], scale=-0.5)
                STT(a_[:, :], a_[:, :], fst[:, 2:3], fgbc[:, :], ALU.mult, ALU.mult, [ba, B_fst, B_fgbc], [ba])
                DMA(out_d[tg * 128:(tg + 1) * 128, :], a_[:, :], "outs%d" % s_, [ba], [B_out[tg]])
            P.wait_all("sync", B_out)
        P.emit(es)
    return nc


def _consts(NTOK):
    ident = np.eye(128, dtype=np.float32)
    s = np.arange(128)[:, None]
    t = np.arange(128)[None, :]
    mf = (s <= t).astype(np.float32)
    mb = (s >= t).astype(np.float32)
    mask = np.concatenate([mf, mb], axis=1)
    sel = np.zeros((8, 8 * 128), np.float32)
    for r in range(8):
        sel[r, r * 128:(r + 1) * 128] = 1.0
    lst = (s < t).astype(np.float32)
    BLK = 512
    NBLK = (4 * NTOK) // BLK + NE - 1
    JMAX = max(1, NTOK // BLK)
    jv = np.tile((BLK * np.arange(JMAX, dtype=np.float32))[None, None, :], (128, NE, 1)).reshape(128, NE * JMAX)
    jb = np.tile((BLK * np.arange(NBLK, dtype=np.float32))[None, :, None], (128, 1, NE)).reshape(128, NBLK * NE)
    ntile = NTOK // 128
    tokid = (np.arange(ntile, dtype=np.int32)[None, :] * 128 + np.arange(128, dtype=np.int32)[:, None]).astype(np.int32)
    pk = (np.arange(128, dtype=np.float32)[:, None] + 128.0 * np.arange(KC, dtype=np.float32)[None, :]).astype(np.float32)
    return dict(c_pk=np.ascontiguousarray(pk), c_ident=ident, c_mask=np.ascontiguousarray(mask), c_sel=sel, c_lst=np.ascontiguousarray(lst),
                c_jv=np.ascontiguousarray(jv), c_jb=np.ascontiguousarray(jb), c_tokid=np.ascontiguousarray(tokid))


def make_in_maps(inputs, n_cores, NB):
    T = inputs["x"].shape[1]
    consts = _consts(NB * T)
    maps = []
    for c in range(n_cores):
        bs = slice(c * NB, (c + 1) * NB)
        m = {k: np.ascontiguousarray(v) for k, v in inputs.items() if k not in ("x", "c", "ctx", "c_ctx")}
        m["x"] = np.ascontiguousarray(inputs["x"][bs])
        m["ctx"] = np.ascontiguousarray(inputs["ctx"][bs])
        m["cc"] = np.ascontiguousarray(np.concatenate([inputs["c"][bs], inputs["c_ctx"][None, :]], axis=0))
        m.update(consts)
        maps.append(m)
    return maps


def kernel(**inputs):
    inputs = {k: np.asarray(v, dtype=np.float32) for k, v in inputs.items()}
    n_cores = 8
    B, T, _ = inputs["x"].shape
    NB = B // n_cores
    nc = build_program(NB, T, inputs["ctx"].shape[1])
    maps = make_in_maps(inputs, n_cores, NB)
    res = run_bass_kernel_spmd(nc, maps, core_ids=list(range(n_cores)))
    out = np.concatenate([r["out"].reshape(NB, T, D) for r in res.results], axis=0)
    return out.astype(np.float32)
```
